# Optimizing a Trainium2 kernel written in Bass

```python
import math
import jax
import jax.numpy as jnp
from jax import lax
import numpy as np

D_MODEL = 1024
BATCH = 8
SEQ = 4096
DEPTH = 2

GRID_W = 64
CTX_LEN = 256
EPS = 1e-6
D_RNN = 1024
RNN_BLOCKS = 8
RNN_BW = D_RNN // RNN_BLOCKS
CONV_W = 4
CONV_LEFT = 2
LRU_C = 8.0
N_HEADS = 8
HEAD_DIM = 64
V_DIM = 2 * HEAD_DIM
ATTN_QK = N_HEADS * 2 * HEAD_DIM
ATTN_V = N_HEADS * V_DIM
Q_BLOCK = 128
ROPE_BASE = 10000.0
N_EXPERTS = 16
N_GROUPS = 4
EXPERTS_PER_GROUP = N_EXPERTS // N_GROUPS
TOP_K = 2
D_EXPERT = 1024
MOE_BLOCK = 256
COL_SPLITS = (D_RNN, ATTN_QK, ATTN_V, ATTN_QK, D_RNN, D_MODEL, D_MODEL)
D_IN = sum(COL_SPLITS)
CTX_LAST_PARTS = 3

kernel_name = 'hybrid_rglru_diffattn_groupmoe_dit'


def _rmsnorm(x, g):
    xf = x.astype(jnp.float32)
    y = xf * lax.rsqrt(jnp.mean(xf * xf, axis=-1, keepdims=True) + EPS)
    return (y * g.astype(jnp.float32)).astype(x.dtype)


def _adaln(cond, w_mod, b_mod):
    m = jax.nn.silu(cond) @ w_mod + b_mod
    return jnp.split(m, 6, axis=-1)


def _modulate(h, shift, scale):
    return h * (1 + scale) + shift


def _split_proj(p, n_parts):
    idx, acc = [], 0
    for s in COL_SPLITS[:n_parts - 1]:
        acc += s
        idx.append(acc)
    return jnp.split(p, idx, axis=-1)


def _axial_rope_tables(rows, dtype):
    n_pairs = HEAD_DIM // 4
    inv = ROPE_BASE ** (-jnp.arange(n_pairs, dtype=jnp.float32) / n_pairs)
    r = jnp.repeat(jnp.arange(rows, dtype=jnp.float32), GRID_W)
    col = jnp.tile(jnp.arange(GRID_W, dtype=jnp.float32), rows)
    ang = jnp.concatenate([r[:, None] * inv, col[:, None] * inv], axis=-1)
    return jnp.cos(ang).astype(dtype), jnp.sin(ang).astype(dtype)


def _apply_rope(t, cos, sin):
    tp = t.reshape(t.shape[:-1] + (HEAD_DIM // 2, 2))
    t1, t2 = tp[..., 0], tp[..., 1]
    cs = cos[None, :, None, None, :]
    sn = sin[None, :, None, None, :]
    return jnp.stack([t1 * cs - t2 * sn, t1 * sn + t2 * cs], axis=-1).reshape(t.shape)


def _centred_dwconv(u, w, b):
    L = u.shape[1]
    up = jnp.pad(u, ((0, 0), (CONV_LEFT, CONV_W - 1 - CONV_LEFT), (0, 0)))
    out = b
    for j in range(CONV_W):
        out = out + up[:, j:j + L] * w[j]
    return out


def _block_diag(u, w, b):
    ub = u.reshape(u.shape[:-1] + (RNN_BLOCKS, RNN_BW))
    return jnp.einsum('blni,nio->blno', ub, w).reshape(u.shape) + b


def _lin_combine(left, right):
    a_l, b_l = left
    a_r, b_r = right
    return a_l * a_r, a_r * b_l + b_r


def _rglru_scan(u, h0, w_a, b_a, w_x, b_x, lam):
    r = jax.nn.sigmoid(_block_diag(u, w_a, b_a).astype(jnp.float32))
    i = jax.nn.sigmoid(_block_diag(u, w_x, b_x).astype(jnp.float32))
    log_a = -LRU_C * r * jax.nn.softplus(-lam.astype(jnp.float32))
    a = jnp.exp(log_a)
    b = jnp.sqrt(-jnp.expm1(2.0 * log_a)) * i * u.astype(jnp.float32)
    b = b.at[:, 0].add(a[:, 0] * h0)
    _, h = lax.associative_scan(_lin_combine, (a, b), axis=1)
    return h


def _bidir_rglru(u_c, u_l, w_a, b_a, w_x, b_x, lam, with_ctx):
    h0 = jnp.zeros((u_l.shape[0], D_RNN), jnp.float32)
    hc_f = _rglru_scan(u_c, h0, w_a[0], b_a[0], w_x[0], b_x[0], lam[0])
    hl_f = _rglru_scan(u_l, hc_f[:, -1], w_a[0], b_a[0], w_x[0], b_x[0], lam[0])
    hc_b = _rglru_scan(jnp.flip(u_c, 1), h0, w_a[1], b_a[1], w_x[1], b_x[1], lam[1])
    hl_b = _rglru_scan(jnp.flip(u_l, 1), hc_b[:, -1], w_a[1], b_a[1], w_x[1], b_x[1], lam[1])
    y_l = (hl_f + jnp.flip(hl_b, 1)).astype(u_l.dtype)
    y_c = (hc_f + jnp.flip(hc_b, 1)).astype(u_c.dtype) if with_ctx else None
    return y_l, y_c


def _diff_attend(q, k, v, lam):
    s = jnp.einsum('bqhcd,bkhcd->bhcqk', q, k, preferred_element_type=jnp.float32) * (HEAD_DIM ** -0.5)
    p = jax.nn.softmax(s, axis=-1)
    p = p[:, :, 0] - lam * p[:, :, 1]
    return jnp.einsum('bhqk,bkhe->bqhe', p.astype(v.dtype), v)


def _latent_diff_attention(q, k_all, v_all, lam):
    B, S = q.shape[:2]
    nb = S // Q_BLOCK
    qb = jnp.moveaxis(q.reshape((B, nb, Q_BLOCK) + q.shape[2:]), 1, 0)
    ob = lax.map(lambda qi: _diff_attend(qi, k_all, v_all, lam), qb)
    return jnp.moveaxis(ob, 0, 1).reshape(B, S, N_HEADS, V_DIM)


def _moe(h, w_router, b_router, w1, w3, w2):
    T, D = h.shape
    logits = jnp.matmul(h, w_router).astype(jnp.float32) + b_router.astype(jnp.float32)
    probs = jax.nn.softmax(logits, axis=-1).reshape(T, N_GROUPS, EXPERTS_PER_GROUP)
    group = jnp.argmax(probs.max(axis=-1), axis=-1)
    in_group = jnp.take_along_axis(probs, group[:, None, None], axis=1)[:, 0]
    top_p, top_local = lax.top_k(in_group, TOP_K)
    expert = (group[:, None] * EXPERTS_PER_GROUP + top_local).reshape(-1).astype(jnp.int32)
    gate = (top_p / jnp.sum(top_p, axis=-1, keepdims=True)).reshape(-1)
    A = T * TOP_K
    order = jnp.argsort(expert)
    sorted_e = expert[order]
    sizes = jnp.bincount(expert, length=N_EXPERTS)
    padded = (sizes + MOE_BLOCK - 1) // MOE_BLOCK * MOE_BLOCK
    starts = jnp.cumsum(sizes) - sizes
    pend = jnp.cumsum(padded)
    pstarts = pend - padded
    dest = pstarts[sorted_e] + jnp.arange(A) - starts[sorted_e]
    n_blocks = -(-A // MOE_BLOCK) + N_EXPERTS
    n_slots = n_blocks * MOE_BLOCK
    slot_tok = jnp.full((n_slots,), T, jnp.int32).at[dest].set((order // TOP_K).astype(jnp.int32))
    slot_gate = jnp.zeros((n_slots,), jnp.float32).at[dest].set(gate[order])
    block_e = jnp.minimum(jnp.searchsorted(pend, jnp.arange(n_blocks) * MOE_BLOCK, side='right'), N_EXPERTS - 1)
    h_pad = jnp.concatenate([h, jnp.zeros((1, D), h.dtype)], axis=0)
    xb = h_pad[slot_tok].reshape(n_blocks, MOE_BLOCK, D)

    def expert_block(args):
        xe, e = args
        return (jax.nn.silu(xe @ w1[e]) * (xe @ w3[e])) @ w2[e]

    yb = lax.map(expert_block, (xb, block_e)).reshape(n_slots, D)
    y = yb * slot_gate[:, None].astype(yb.dtype)
    return jnp.zeros((T + 1, D), h.dtype).at[slot_tok].add(y)[:T]


def setup_inputs(seed: int = 0) -> dict:
    key = jax.random.key(seed)
    ks = jax.random.split(key, 32)
    f32 = jnp.float32
    D = D_MODEL

    def nrm(k, shape, s):
        return jax.random.normal(k, shape, f32) * s

    a8 = jax.random.uniform(ks[15], (DEPTH, 2, D_RNN), f32, 0.9, 0.999)
    a = a8 ** (1.0 / LRU_C)
    return {
        'x': nrm(ks[0], (BATCH, SEQ, D), 1.0),
        'c': nrm(ks[1], (BATCH, D), 1.0),
        'ctx': nrm(ks[2], (BATCH, CTX_LEN, D), 1.0),
        'c_ctx': nrm(ks[3], (D,), 1.0),
        'w_mod': nrm(ks[4], (DEPTH, D, 6 * D), 0.5 * D ** -0.5),
        'b_mod': nrm(ks[5], (DEPTH, 6 * D), 0.02),
        'g_norm1': 1.0 + nrm(ks[6], (DEPTH, D), 0.02),
        'g_norm2': 1.0 + nrm(ks[7], (DEPTH, D), 0.02),
        'w_in': nrm(ks[8], (DEPTH, D, D_IN), D ** -0.5),
        'conv_w': nrm(ks[9], (DEPTH, CONV_W, D_RNN), CONV_W ** -0.5),
        'conv_b': nrm(ks[10], (DEPTH, D_RNN), 0.02),
        'lru_wa': nrm(ks[11], (DEPTH, 2, RNN_BLOCKS, RNN_BW, RNN_BW), RNN_BW ** -0.5),
        'lru_ba': nrm(ks[12], (DEPTH, 2, D_RNN), 0.02),
        'lru_wx': nrm(ks[13], (DEPTH, 2, RNN_BLOCKS, RNN_BW, RNN_BW), RNN_BW ** -0.5),
        'lru_bx': nrm(ks[14], (DEPTH, 2, D_RNN), 0.02),
        'lru_lambda': jnp.log(a) - jnp.log1p(-a),
        'diff_lambda': nrm(ks[16], (DEPTH, 4, HEAD_DIM), 0.1),
        'g_subln': 1.0 + nrm(ks[17], (DEPTH, V_DIM), 0.02),
        'w_rnn_proj': nrm(ks[18], (DEPTH, D_RNN, D), D_RNN ** -0.5),
        'w_attn_proj': nrm(ks[19], (DEPTH, ATTN_V, D), ATTN_V ** -0.5),
        'w_o': nrm(ks[20], (DEPTH, D, D), D ** -0.5),
        'w_router': nrm(ks[21], (D, N_EXPERTS), D ** -0.5),
        'b_router': nrm(ks[22], (N_EXPERTS,), 0.01),
        'w_e1': nrm(ks[23], (DEPTH, N_EXPERTS, D, D_EXPERT), D ** -0.5),
        'w_e3': nrm(ks[24], (DEPTH, N_EXPERTS, D, D_EXPERT), D ** -0.5),
        'w_e2': nrm(ks[25], (DEPTH, N_EXPERTS, D_EXPERT, D), D_EXPERT ** -0.5),
        'g_final': 1.0 + nrm(ks[26], (D,), 0.02),
    }


def reference(x, c, ctx, c_ctx, w_mod, b_mod, g_norm1, g_norm2, w_in, conv_w, conv_b,
              lru_wa, lru_ba, lru_wx, lru_bx, lru_lambda, diff_lambda, g_subln,
              w_rnn_proj, w_attn_proj, w_o, w_router, b_router, w_e1, w_e3, w_e2, g_final):
    B, S, D = x.shape
    Lc = ctx.shape[1]
    rows = S // GRID_W
    cos, sin = _axial_rope_tables(rows, x.dtype)

    def head5(t):
        return t.reshape(t.shape[:2] + (N_HEADS, 2, HEAD_DIM))

    def headv(t):
        return t.reshape(t.shape[:2] + (N_HEADS, V_DIM))

    xc = ctx
    for l in range(DEPTH):
        last = l == DEPTH - 1
        lam_init = 0.8 - 0.6 * math.exp(-0.3 * l)
        sh1, sc1, gt1, sh2, sc2, gt2 = [m[:, None, :] for m in _adaln(c, w_mod[l], b_mod[l])]
        csh1, csc1, cgt1, csh2, csc2, cgt2 = _adaln(c_ctx, w_mod[l], b_mod[l])

        h = _modulate(_rmsnorm(x, g_norm1[l]), sh1, sc1)
        hc = _modulate(_rmsnorm(xc, g_norm1[l]), csh1, csc1)
        rx, k, v, q, rg, g_r, g_a = _split_proj(h @ w_in[l], 7)
        n_ctx_parts = CTX_LAST_PARTS if last else 7
        ctx_parts = _split_proj(hc @ w_in[l][:, :sum(COL_SPLITS[:n_ctx_parts])], n_ctx_parts)
        rx_c, k_c, v_c = ctx_parts[0], ctx_parts[1], ctx_parts[2]

        lq1, lk1, lq2, lk2 = diff_lambda[l].astype(jnp.float32)
        lam = jnp.exp(jnp.sum(lq1 * lk1)) - jnp.exp(jnp.sum(lq2 * lk2)) + lam_init
        q_l = _apply_rope(head5(q), cos, sin)
        k_l = _apply_rope(head5(k), cos, sin)
        k_c5 = head5(k_c)
        v_c4 = headv(v_c)
        k_all = jnp.concatenate([k_c5, k_l], axis=1)
        v_all = jnp.concatenate([v_c4, headv(v)], axis=1)
        o_a = _latent_diff_attention(q_l, k_all, v_all, lam)
        o_a = (_rmsnorm(o_a, g_subln[l]) * (1.0 - lam_init)).reshape(B, S, ATTN_V) @ w_attn_proj[l]

        u_l = _centred_dwconv(rx, conv_w[l], conv_b[l])
        u_c = _centred_dwconv(rx_c, conv_w[l], conv_b[l])
        y_l, y_c = _bidir_rglru(u_c, u_l, lru_wa[l], lru_ba[l], lru_wx[l], lru_bx[l], lru_lambda[l], not last)
        o_r = (y_l * jax.nn.gelu(rg)) @ w_rnn_proj[l]

        x = x + gt1 * ((jax.nn.sigmoid(g_r) * o_r + jax.nn.sigmoid(g_a) * o_a) @ w_o[l])
        if not last:
            q_c, rg_c, g_r_c, g_a_c = ctx_parts[3], ctx_parts[4], ctx_parts[5], ctx_parts[6]
            o_ac = _diff_attend(head5(q_c), k_c5, v_c4, lam)
            o_ac = (_rmsnorm(o_ac, g_subln[l]) * (1.0 - lam_init)).reshape(B, Lc, ATTN_V) @ w_attn_proj[l]
            o_rc = (y_c * jax.nn.gelu(rg_c)) @ w_rnn_proj[l]
            xc = xc + cgt1 * ((jax.nn.sigmoid(g_r_c) * o_rc + jax.nn.sigmoid(g_a_c) * o_ac) @ w_o[l])

        h2 = _modulate(_rmsnorm(x, g_norm2[l]), sh2, sc2).reshape(B * S, D)
        if not last:
            h2c = _modulate(_rmsnorm(xc, g_norm2[l]), csh2, csc2).reshape(B * Lc, D)
            y = _moe(jnp.concatenate([h2, h2c], axis=0), w_router, b_router, w_e1[l], w_e3[l], w_e2[l])
            xc = xc + cgt2 * y[B * S:].reshape(B, Lc, D)
            y_lat = y[:B * S]
        else:
            y_lat = _moe(h2, w_router, b_router, w_e1[l], w_e3[l], w_e2[l])
        x = x + gt2 * y_lat.reshape(B, S, D)

    return _rmsnorm(x, g_final)
```

```python
import os
import math
import numpy as np
import concourse.bass as bass
import concourse.mybir as mybir
from concourse.bass_utils import run_bass_kernel_spmd

F32 = mybir.dt.float32
BF16 = mybir.dt.bfloat16
AF = mybir.ActivationFunctionType
ALU = mybir.AluOpType
AX = mybir.AxisListType

D = 1024
LC = 256
SEQ = 4096
TOK = LC + SEQ
NT = TOK // 128
DEPTH = 2
NE = 16
EPS = 1e-6
NEXT = 9216
DBG = {}
MOE_SPARSE = True
I32 = mybir.dt.int32
NBMAX = 25
BSL = 1024


class Buf:
    __slots__ = ("name", "writer", "readers", "lsem", "ssem", "pend")

    def __init__(self, name):
        self.name = name
        self.writer = None
        self.readers = []
        self.lsem = None
        self.ssem = None
        self.pend = False


class Sched:
    COMPUTE = ("pe", "act", "dve", "pool")

    def __init__(self, nc):
        self.nc = nc
        self.eng = {"pe": nc.tensor, "act": nc.scalar, "dve": nc.vector,
                    "pool": nc.gpsimd, "sp": nc.sync}
        self.csem = {}
        self.ccnt = {}
        self._stack = []
        for e in self.COMPUTE:
            g = nc.semaphore("cs_" + e)
            self.csem[e] = g.__enter__()
            self._stack.append(g)
            self.ccnt[e] = 0
        self.seen = {e: {} for e in self.eng}
        self.pe_pending = []
        self.pe_pending_w = []
        self.dpool = []
        self.dall = []
        self.dholders = []
        self.ninstr = {e: 0 for e in self.eng}

    def _getdsem(self, q):
        kind = "sw" if q == "pool" else "hw"
        for i, d in enumerate(self.dpool):
            if d["kind"] == kind:
                return self.dpool.pop(i)
        i = len(self.dall)
        g = self.nc.semaphore("ds_%d" % i)
        s = g.__enter__()
        self._stack.append(g)
        d = {"sem": s, "cnt": 0, "key": i, "kind": kind}
        self.dall.append(d)
        return d

    def _wait(self, consumer, key, sem, val):
        d = self.seen[consumer]
        if d.get(key, 0) >= val:
            return
        d[key] = val
        self.eng[consumer].wait_ge(sem, val)
        self.ninstr[consumer] += 1

    def _wait_rec(self, consumer, rec):
        if rec is None:
            return
        if rec[0] == "c":
            _, e, seq = rec
            if e == "pe" and consumer == "pe":
                return
            assert seq is not None, "dependency on unsignaled instr"
            self._wait(consumer, ("c", e), self.csem[e], seq)
        else:
            _, sem, cnt, key = rec
            self._wait(consumer, ("d", key), sem, cnt)

    def _deps(self, consumer, reads, writes):
        for b in reads:
            if b.writer is not None and b.writer[0] == "c" and b.writer[2] is None:
                assert consumer == "pe", "read of %s whose PE writer is unsignaled" % b.name
            self._wait_rec(consumer, b.writer)
        for b in writes:
            assert not (b.pend and consumer != "pe"), "write to %s with pending unsignaled PE read" % b.name
            if b.writer is not None and b.writer[0] == "c" and b.writer[2] is None:
                assert consumer == "pe", "write to %s whose PE writer is unsignaled" % b.name
            self._wait_rec(consumer, b.writer)
            for r in b.readers:
                self._wait_rec(consumer, r)

    @staticmethod
    def _trim(b):
        if len(b.readers) > 8:
            last = {}
            for r in b.readers:
                k = (r[0], r[1]) if r[0] == "c" else (r[0], r[3])
                last[k] = r
            b.readers = list(last.values())

    def op(self, e, fn, reads=(), writes=(), sig=True):
        self._deps(e, reads, writes)
        ins = fn()
        self.ninstr[e] += 1
        if sig:
            self.ccnt[e] += 1
            seq = self.ccnt[e]
            ins.then_inc(self.csem[e], 1)
            rec = ("c", e, seq)
            if e == "pe":
                for b in self.pe_pending:
                    b.readers.append(rec)
                    b.pend = False
                self.pe_pending = []
                for b in self.pe_pending_w:
                    b.writer = rec
                self.pe_pending_w = []
        else:
            assert e == "pe"
            rec = ("c", e, None)
        for b in writes:
            b.writer = rec
            b.readers = []
            if rec[2] is None:
                self.pe_pending_w.append(b)
        for b in reads:
            if b in writes:
                continue
            if rec[2] is None:
                if not b.pend:
                    b.pend = True
                    self.pe_pending.append(b)
            else:
                b.readers.append(rec)
                self._trim(b)
        return ins

    def dma(self, q, out, in_, reads=(), writes=(), **kw):
        self._deps(q, reads, writes)
        ins = self.eng[q].dma_start(out=out, in_=in_, **kw)
        self.ninstr[q] += 1
        if writes:
            b = writes[0]
            if b.lsem is None or b.lsem["kind"] != ("sw" if q == "pool" else "hw"):
                b.lsem = self._getdsem(q)
                self.dholders.append(b)
            d = b.lsem
        else:
            b = reads[0]
            if b.ssem is None or b.ssem["kind"] != ("sw" if q == "pool" else "hw"):
                b.ssem = self._getdsem(q)
                self.dholders.append(b)
            d = b.ssem
        d["cnt"] += 16
        ins.then_inc(d["sem"], 16)
        rec = ("d", d["sem"], d["cnt"], d["key"])
        for b in writes:
            b.writer = rec
            b.readers = []
        for b in reads:
            b.readers.append(rec)
            self._trim(b)
        return ins

    def idma(self, out, out_offset, in_, in_offset, reads=(), writes=()):
        q = "pool"
        self._deps(q, reads, writes)
        ins = self.nc.gpsimd.indirect_dma_start(out=out, out_offset=out_offset, in_=in_, in_offset=in_offset)
        self.ninstr[q] += 1
        b = writes[0]
        if b.lsem is None or b.lsem["kind"] != "sw":
            b.lsem = self._getdsem(q)
            self.dholders.append(b)
        d = b.lsem
        d["cnt"] += 16
        ins.then_inc(d["sem"], 16)
        rec = ("d", d["sem"], d["cnt"], d["key"])
        for b in writes:
            b.writer = rec
            b.readers = []
        for b in reads:
            b.readers.append(rec)
            self._trim(b)
        return ins

    def wait_buf(self, consumer, b):
        self._wait_rec(consumer, b.writer)
        for r in b.readers:
            self._wait_rec(consumer, r)

    def barrier(self, engines=None):
        assert not self.pe_pending and not self.pe_pending_w, "unsignaled PE work at barrier"
        self.marks = getattr(self, "marks", []) + [dict(self.ccnt)]
        for q in (engines or self.eng):
            for e in self.COMPUTE:
                if self.ccnt[e] > 0:
                    self._wait(q, ("c", e), self.csem[e], self.ccnt[e])
            for d in self.dall:
                if d["cnt"] > 0:
                    self._wait(q, ("d", d["key"]), d["sem"], d["cnt"])
        for b in self.dholders:
            b.lsem = None
            b.ssem = None
        self.dholders = []
        self.dpool = list(self.dall)

    def close(self):
        for g in reversed(self._stack):
            g.__exit__(None, None, None)


class Scope:
    def __init__(self, nc, tag):
        self.nc = nc
        self.tag = tag
        self.gs = []
        self.n = 0

    def sb(self, name, shape, dt):
        g = self.nc.sbuf_tensor("%s_%s" % (name, self.tag), list(shape), dt)
        t = g.__enter__()
        self.gs.append(g)
        return t, Buf(name)

    def ps(self, name, shape, dt):
        g = self.nc.psum_tensor("%s_%s" % (name, self.tag), list(shape), dt)
        t = g.__enter__()
        self.gs.append(g)
        return t, Buf(name)

    def close(self):
        for g in reversed(self.gs):
            g.__exit__(None, None, None)
        self.gs = []


def token_chunks(last):
    ch = []
    if not last:
        ch.append((0, LC, True))
    for i in range(SEQ // 512):
        ch.append((LC + i * 512, 512, False))
    return ch


def build(debug_stop=None, n_layers=DEPTH):
    nc = bass.Bass("TRN2", target_bir_lowering=False)
    S = Sched(nc)
    dbg = debug_stop is not None
    skind = "ExternalOutput" if dbg else "Internal"

    def din(name, shape, dt=F32):
        return nc.dram_tensor(name, list(shape), dt, kind="ExternalInput").ap()

    def dscr(name, shape, dt):
        return nc.dram_tensor(name, list(shape), dt, kind=skind).ap(), Buf(name)

    x_in = din("x_in", [TOK, D])
    cc_d = din("cc", [128, 8, 2])
    w_mod = din("w_mod", [DEPTH, D, 6 * D])
    b_mod = din("b_mod", [DEPTH, 6 * D])
    bmodP_d = din("bmodP", [DEPTH, 128, 4, 8, 2])
    gn_d = din("gn", [DEPTH, 128, 2, 8, 2])
    w_in = din("w_in_ext", [DEPTH, D, NEXT])
    ctab_d = din("ctab", [128, SEQ])
    stab_d = din("stab", [128, SEQ])
    ident_d = din("ident", [128, 128])
    cw_d = din("cw", [DEPTH, 128, 8, 4])
    cb_d = din("cb", [DEPTH, 128, 8])
    lwa_d = din("lwa", [DEPTH, 128, 16 * 128])
    lwx_d = din("lwx", [DEPTH, 128, 16 * 128])
    lba_d = din("lba", [DEPTH, 128, 16])
    lbx_d = din("lbx", [DEPTH, 128, 16])
    llam_d = din("llam", [DEPTH, 128, 16])
    dlam_d = din("dlam", [DEPTH, 256])
    gsub_d = din("gsub", [DEPTH, 128])
    wrp_d = din("w_rnn_proj", [DEPTH, D, D])
    wap_d = din("w_attn_proj", [DEPTH, D, D])
    wo_d = din("w_o", [DEPTH, D, D])
    wrt_d = din("w_router", [D, NE])
    brt_d = din("b_router", [NE])
    we1_d = din("w_e1", [DEPTH, NE, D, D])
    we3_d = din("w_e3", [DEPTH, NE, D, D])
    we2_d = din("w_e2", [DEPTH, NE, D, D])
    gfin_d = din("g_final", [D])
    utri_d = din("utri", [128, 128])
    onesm_d = din("onesm", [128, 128])
    prow_d = din("prow", [128, 8])
    thr_d = din("thr", [128, 64])
    out_d = nc.dram_tensor("out", [SEQ, D], F32, kind="ExternalOutput").ap()
    Bout = Buf("out")

    XMID, BXMID = dscr("XMID", [TOK, D], F32)
    XRES, BXRES = dscr("XRES", [TOK, D], F32)
    RX, BRX = dscr("RX", [8, 128, TOK], F32)
    KT, BKT = dscr("KT", [8, 128, TOK], BF16)
    QT, BQT = dscr("QT", [8, 128, TOK], BF16)
    VV, BVV = dscr("VV", [TOK, D], BF16)
    GRG, BGRG = dscr("GRG", [8, 128, TOK], BF16)
    SGR, BSGR = dscr("SGR", [8, 128, TOK], BF16)
    SGA, BSGA = dscr("SGA", [8, 128, TOK], BF16)
    OAT, BOAT = dscr("OAT", [8, 128, TOK], BF16)
    ZT, BZT = dscr("ZT", [8, 128, TOK], BF16)
    H2T, BH2T = dscr("H2T", [128, 8, TOK], BF16)
    H2TOK, BH2TOK = dscr("H2TOK", [TOK, D], BF16)
    XS, BXS = dscr("XS", [NBMAX * BSL, D], BF16)
    YS, BYS = dscr("YS", [NBMAX * BSL, D], F32)

    P = Scope(nc, "P")
    identb, Bident = P.sb("identb", [128, 128], BF16)
    sil, Bsil = P.sb("sil", [128, 8, 2], F32)
    gt1bc, Bgt1 = P.sb("gt1bc", [128, 2, D], F32)
    gt2bc, Bgt2 = P.sb("gt2bc", [128, 2, D], F32)
    modP, BmodP = P.sb("modP", [128, 4, 8, 2], F32)
    AA, BAA = P.sb("AA", [128, 2, 8, 2], F32)
    logits, Blog = P.sb("logits", [128, NT, NE], F32)
    gates, Bgates = P.sb("gates", [128, NT, NE], F32)
    brtbc, Bbrt = P.sb("brtbc", [128, NE], F32)
    wrt, Bwrt = P.sb("wrt", [128, 8, NE], BF16)

    utri16, Butri = P.sb("utri16", [128, 128], BF16)
    ones16m, Bonesm = P.sb("ones16m", [128, 128], BF16)
    prow, Bprow = P.sb("prow", [128, 8], F32)
    thr, Bthr = P.sb("thr", [128, 64], F32)
    S.dma("pool", utri16[:], utri_d, writes=[Butri])
    S.dma("pool", ones16m[:], onesm_d, writes=[Bonesm])
    S.dma("sp", prow[:], prow_d, writes=[Bprow])
    S.dma("sp", thr[:], thr_d, writes=[Bthr])
    S.dma("pool", identb[:], ident_d, writes=[Bident])
    S.dma("sp", sil[:], cc_d, writes=[Bsil])
    S.dma("sp", brtbc[:], brt_d.partition_broadcast(128), writes=[Bbrt])
    S.dma("pool", wrt[:], wrt_d.rearrange("(kc p) e -> p kc e", p=128), writes=[Bwrt])
    S.op("act", lambda: nc.scalar.activation(out=sil[:], in_=sil[:], func=AF.Silu), reads=[Bsil], writes=[Bsil])

    def rmsnorm_T(sc, x_ap, Bx, li, which, jj, dst_fn, Bdst, PT, BPT, tmp):
        junk, Bjunk, ss, Bss, xn, Bxn = tmp
        S.op("act", lambda: nc.scalar.activation(out=junk[:], in_=x_ap, func=AF.Square, accum_out=ss[:, 0:1]),
             reads=[Bx], writes=[Bjunk, Bss])
        S.op("act", lambda: nc.scalar.activation(out=ss[:, 1:2], in_=ss[:, 0:1], func=AF.Sqrt, scale=1.0 / D, bias=EPS),
             reads=[Bss], writes=[Bss])
        S.op("dve", lambda: nc.vector.reciprocal(out=ss[:, 2:3], in_=ss[:, 1:2]), reads=[Bss], writes=[Bss])
        S.op("act", lambda: nc.scalar.activation(out=xn[:], in_=x_ap, func=AF.Identity, scale=ss[:, 2:3]),
             reads=[Bx, Bss], writes=[Bxn])
        for kc in range(8):
            S.op("pe", lambda kc=kc: nc.tensor.transpose(out=PT[:, kc * 128:(kc + 1) * 128],
                                                          in_=xn[:, kc * 128:(kc + 1) * 128], identity=identb[:]),
                 reads=[Bxn, Bident], writes=[BPT], sig=(kc == 7))
        for kc in range(8):
            S.op("dve", lambda kc=kc: nc.vector.tensor_scalar(
                out=dst_fn(kc), in0=PT[:, kc * 128:(kc + 1) * 128],
                scalar1=AA[:, which, kc, jj:jj + 1], scalar2=modP[:, 2 * which, kc, jj:jj + 1],
                op0=ALU.mult, op1=ALU.add), reads=[BPT, BAA, BmodP], writes=[Bdst])

    def stop_here(name):
        return dbg and debug_stop == name

    done = False
    for li in range(n_layers):
        if li in DBG.get("skip_layers", ()):
            continue
        last = li == DEPTH - 1
        lam_init = 0.8 - 0.6 * math.exp(-0.3 * li)
        xcur, Bxcur = (x_in, Buf("x_in")) if li == 0 else (XRES, BXRES)
        chunks = token_chunks(last)

        skipAB = DBG.get("skipAB", False)
        sc = Scope(nc, "A%d" % li)
        wm = [sc.sb("wm%d" % i, [128, 8, 512], F32) for i in range(2)]
        silbc, Bsilbc = sc.sb("silbc", [128, 8, 2, 128], F32)
        for kc in range(8):
            for j in range(2):
                S.op("dve", lambda kc=kc, j=j: nc.vector.tensor_copy(
                    out=silbc[:, kc, j, :], in_=sil[:, kc, j:j + 1].to_broadcast([128, 128])),
                    reads=[Bsil], writes=[Bsilbc])
        bmg, Bbmg = sc.sb("bmg", [128, 2048], F32)
        bmp, Bbmp = sc.sb("bmp", [128, 4, 8, 2], F32)
        gnt, Bgnt = sc.sb("gnt", [128, 2, 8, 2], F32)
        pmod, Bpmod = sc.ps("pmod", [128, 512], F32)
        pg = [sc.ps("pg%d" % i, [128, 512], F32) for i in range(2)]
        S.dma("sp", bmg[:, 0:1024], b_mod[li, 2048:3072].partition_broadcast(128), writes=[Bbmg])
        S.dma("sp", bmg[:, 1024:2048], b_mod[li, 5120:6144].partition_broadcast(128), writes=[Bbmg])
        S.dma("sp", bmp[:], bmodP_d[li], writes=[Bbmp])
        S.dma("sp", gnt[:], gn_d[li], writes=[Bgnt])
        wm_src = w_mod[li].rearrange("(kc p) n -> p kc n", p=128)
        ppi = 0
        gi = 0
        for blk in range(12):
            wt, Bw = wm[blk % 2]
            S.dma("sp", wt[:], wm_src[:, :, blk * 512:(blk + 1) * 512], writes=[Bw])
            if blk in (4, 5, 10, 11):
                gdst, Bgd = (gt1bc, Bgt1) if blk < 6 else (gt2bc, Bgt2)
                half = blk % 2
                bo = (0 if blk < 6 else 1024) + half * 512
                for j in range(2):
                    pt_, Bp = pg[gi % 2]
                    gi += 1
                    for kc in range(8):
                        S.op("pe", lambda kc=kc, j=j, pt_=pt_, wt=wt: nc.tensor.matmul(
                            pt_[:, :], lhsT=silbc[:, kc, j, :], rhs=wt[:, kc, :], start=(kc == 0), stop=(kc == 7)),
                            reads=[Bsilbc, Bw], writes=[Bp], sig=(kc == 7))
                    S.op("dve", lambda j=j, pt_=pt_, gdst=gdst, half=half, bo=bo: nc.vector.tensor_tensor(
                        out=gdst[:, j, half * 512:(half + 1) * 512], in0=pt_[:, :], in1=bmg[:, bo:bo + 512], op=ALU.add),
                        reads=[Bp, Bbmg], writes=[Bgd])
            else:
                for cch in range(4):
                    for kc in range(8):
                        S.op("pe", lambda kc=kc, cch=cch, wt=wt, ppi=ppi: nc.tensor.matmul(
                            pmod[:, 2 * ppi:2 * ppi + 2], lhsT=wt[:, kc, cch * 128:(cch + 1) * 128], rhs=sil[:, kc, :],
                            start=(kc == 0), stop=(kc == 7)),
                            reads=[Bw, Bsil], writes=[Bpmod], sig=(kc == 7))
                    ppi += 1
        assert ppi == 32
        if True:
          S.op("dve", lambda: nc.vector.tensor_tensor(
            out=modP[:].rearrange("p a b c -> p (a b c)"), in0=pmod[:, 0:64],
            in1=bmp[:].rearrange("p a b c -> p (a b c)"), op=ALU.add), reads=[Bpmod, Bbmp], writes=[BmodP])
        for which in range(2):
            S.op("dve", lambda which=which: nc.vector.scalar_tensor_tensor(
                out=AA[:, which].rearrange("p b c -> p (b c)"),
                in0=modP[:, 2 * which + 1].rearrange("p b c -> p (b c)"), scalar=1.0,
                in1=gnt[:, which].rearrange("p b c -> p (b c)"), op0=ALU.add, op1=ALU.mult),
                reads=[BmodP, Bgnt], writes=[BAA])
        S.barrier()
        sc.close()
        if stop_here("A%d" % li):
            done = True
            break

        scB = Scope(nc, "B%d" % li)
        hT, BhT = scB.sb("hT", [128, 8, TOK], BF16)
        sc = Scope(nc, "B1%d" % li)
        xt = [sc.sb("xt%d" % i, [128, D], F32) for i in range(3)]
        tmps = [(sc.sb("junk%d" % i, [128, D], BF16) + sc.sb("ss%d" % i, [128, 4], F32) + sc.sb("xn%d" % i, [128, D], BF16))
                for i in range(2)]
        PTs = [sc.ps("PT%d" % i, [128, D], BF16) for i in range(2)]
        for t in range(0 if skipAB else NT):
            if last and False:
                pass
            xtt, Bxt = xt[t % 3]
            S.dma("sp", xtt[:], xcur[t * 128:(t + 1) * 128, :], reads=[Bxcur], writes=[Bxt])
            PT, BPT = PTs[t % 2]
            jj = 1 if t < 2 else 0
            rmsnorm_T(sc, xtt[:], Bxt, li, 0, jj,
                      lambda kc, t=t: hT[:, kc, t * 128:(t + 1) * 128], BhT, PT, BPT, tmps[t % 2])
        S.barrier()
        sc.close()

        sc = Scope(nc, "B2%d" % li)
        wb = [sc.sb("wb%d" % i, [128, 8, 512], BF16) for i in range(4)]
        ctab, Bctab = sc.sb("ctab", [128, SEQ], BF16)
        stab, Bstab = sc.sb("stab", [128, SEQ], BF16)
        st32, Bst32 = sc.sb("st32", [128, TOK], F32)
        st16 = [sc.sb("st16_%d" % i, [128, TOK], BF16) for i in range(2)]
        t1s = [sc.sb("t1_%d" % i, [128, 512], F32) for i in range(2)]
        t2s = [sc.sb("t2_%d" % i, [128, 512], F32) for i in range(2)]
        stv = [sc.sb("stv%d" % i, [128, 512], BF16) for i in range(4)]
        PB = [sc.ps("PB%d" % i, [128, 512], F32) for i in range(8)]
        S.dma("pool", ctab[:], ctab_d, writes=[Bctab])
        S.dma("pool", stab[:], stab_d, writes=[Bstab])
        win_src = w_in[li].rearrange("(kc p) n -> p kc n", p=128)
        jobs = [(0,), (1,), (2, 14), (3, 15), (4,), (5,), (6, 16), (7, 17), (8,), (9,), (10,), (11,), (12,), (13,)]
        loads = [b for jb in jobs for b in jb]
        slot_of = {}
        nload = [0]

        def load_next():
            if nload[0] < len(loads):
                b = loads[nload[0]]
                wt, Bw = wb[nload[0] % 4]
                slot_of[b] = nload[0] % 4
                S.dma("pool", wt[:], win_src[:, :, b * 512:(b + 1) * 512], writes=[Bw])
                nload[0] += 1

        cum = []
        acc_ = 0
        for jb in jobs:
            acc_ += len(jb)
            cum.append(acc_)
        pbi = [0]
        s16i = [0]
        tti = [0]
        evi = [0]

        def nextpb():
            r = PB[pbi[0] % 8]
            pbi[0] += 1
            return r

        for ji, jb in enumerate([] if skipAB else jobs):
            while nload[0] < cum[min(ji + 1, len(jobs) - 1)]:
                load_next()
            b0 = jb[0]
            part = b0 // 2
            w0, Bw0 = wb[slot_of[b0]]
            if part == 2:
                tiles = range(NT)
                for tt in tiles:
                    pt_, Bp = nextpb()
                    for kc in range(8):
                        S.op("pe", lambda kc=kc, tt=tt, pt_=pt_, w0=w0: nc.tensor.matmul(
                            pt_[:, :], lhsT=hT[:, kc, tt * 128:(tt + 1) * 128], rhs=w0[:, kc, :],
                            start=(kc == 0), stop=(kc == 7)), reads=[BhT, Bw0], writes=[Bp], sig=(kc == 7))
                    sv, Bsv = stv[tt % 4]
                    eng = "act" if tt % 2 == 0 else "dve"
                    if eng == "act":
                        S.op("act", lambda sv=sv, pt_=pt_: nc.scalar.copy(out=sv[:], in_=pt_[:, :]), reads=[Bp], writes=[Bsv])
                    else:
                        S.op("dve", lambda sv=sv, pt_=pt_: nc.vector.tensor_copy(out=sv[:], in_=pt_[:, :]), reads=[Bp], writes=[Bsv])
                    c0 = (b0 - 4) * 512
                    S.dma("sp", VV[tt * 128:(tt + 1) * 128, c0:c0 + 512], sv[:], reads=[Bsv], writes=[BVV])
                continue
            need_ctx = (not last) or part in (0, 1)
            for cch in range(4):
                fc = (b0 % 2) * 4 + cch
                if part in (1, 3):
                    w1_, Bw1 = wb[slot_of[jb[1]]]
                    so, Bso = st16[s16i[0] % 2]
                    s16i[0] += 1
                    for (tok0, n, isctx) in token_chunks(False):
                        if isctx and not need_ctx:
                            continue
                        pk, Bpk = nextpb()
                        for kc in range(8):
                            S.op("pe", lambda kc=kc, pk=pk, tok0=tok0, n=n, cch=cch: nc.tensor.matmul(
                                pk[:, 0:n], lhsT=w0[:, kc, cch * 128:(cch + 1) * 128], rhs=hT[:, kc, tok0:tok0 + n],
                                start=(kc == 0), stop=(kc == 7)), reads=[BhT, Bw0], writes=[Bpk], sig=(kc == 7))
                        if isctx:
                            S.op("act", lambda pk=pk, so=so, n=n: nc.scalar.copy(out=so[:, 0:n], in_=pk[:, 0:n]),
                                 reads=[Bpk], writes=[Bso])
                            continue
                        pks, Bpks = nextpb()
                        for kc in range(8):
                            S.op("pe", lambda kc=kc, pks=pks, tok0=tok0, n=n, cch=cch: nc.tensor.matmul(
                                pks[:, 0:n], lhsT=w1_[:, kc, cch * 128:(cch + 1) * 128], rhs=hT[:, kc, tok0:tok0 + n],
                                start=(kc == 0), stop=(kc == 7)), reads=[BhT, Bw1], writes=[Bpks], sig=(kc == 7))
                        t1, Bt1 = t1s[tti[0] % 2]
                        t2, Bt2 = t2s[tti[0] % 2]
                        tti[0] += 1
                        l0 = tok0 - LC
                        S.op("dve", lambda t1=t1, pk=pk, l0=l0: nc.vector.tensor_tensor(
                            out=t1[:], in0=pk[:, :], in1=ctab[:, l0:l0 + 512], op=ALU.mult), reads=[Bpk, Bctab], writes=[Bt1])
                        S.op("dve", lambda t2=t2, pks=pks, l0=l0: nc.vector.tensor_tensor(
                            out=t2[:], in0=pks[:, :], in1=stab[:, l0:l0 + 512], op=ALU.mult), reads=[Bpks, Bstab], writes=[Bt2])
                        S.op("pool", lambda t1=t1, t2=t2, so=so, tok0=tok0: nc.gpsimd.tensor_tensor(
                            out=so[:, tok0:tok0 + 512], in0=t1[:], in1=t2[:], op=ALU.add), reads=[Bt1, Bt2], writes=[Bso])
                    dst, Bdst = (KT, BKT) if part == 1 else (QT, BQT)
                    a0 = 0 if need_ctx else LC
                    S.dma("sp", dst[fc, :, a0:TOK], so[:, a0:TOK], reads=[Bso], writes=[Bdst])
                else:
                    if part == 0:
                        so, Bso = st32, Bst32
                    else:
                        so, Bso = st16[s16i[0] % 2]
                        s16i[0] += 1
                    for (tok0, n, isctx) in token_chunks(False):
                        if isctx and not need_ctx:
                            continue
                        pk, Bpk = nextpb()
                        for kc in range(8):
                            S.op("pe", lambda kc=kc, pk=pk, tok0=tok0, n=n, cch=cch: nc.tensor.matmul(
                                pk[:, 0:n], lhsT=w0[:, kc, cch * 128:(cch + 1) * 128], rhs=hT[:, kc, tok0:tok0 + n],
                                start=(kc == 0), stop=(kc == 7)), reads=[BhT, Bw0], writes=[Bpk], sig=(kc == 7))
                        if part == 0:
                            evi[0] += 1
                            if evi[0] % 2 == 0:
                                S.op("dve", lambda pk=pk, so=so, tok0=tok0, n=n: nc.vector.tensor_copy(
                                    out=so[:, tok0:tok0 + n], in_=pk[:, 0:n]), reads=[Bpk], writes=[Bso])
                            else:
                                S.op("act", lambda pk=pk, so=so, tok0=tok0, n=n: nc.scalar.copy(
                                    out=so[:, tok0:tok0 + n], in_=pk[:, 0:n]), reads=[Bpk], writes=[Bso])
                        else:
                            fn = AF.Gelu_apprx_tanh if part == 4 else AF.Sigmoid
                            S.op("act", lambda pk=pk, so=so, tok0=tok0, n=n, fn=fn: nc.scalar.activation(
                                out=so[:, tok0:tok0 + n], in_=pk[:, 0:n], func=fn), reads=[Bpk], writes=[Bso])
                    dst, Bdst = {0: (RX, BRX), 4: (GRG, BGRG), 5: (SGR, BSGR), 6: (SGA, BSGA)}[part]
                    a0 = 0 if need_ctx else LC
                    S.dma("sp", dst[fc, :, a0:TOK], so[:, a0:TOK], reads=[Bso], writes=[Bdst])
        S.barrier()
        sc.close()
        scB.close()
        if stop_here("B%d" % li):
            done = True
            break

        sc = Scope(nc, "C%d" % li)
        ktb = [sc.sb("ktb%d" % i, [128, TOK], BF16) for i in range(2)]
        qtb = [[sc.sb("qtb%d_%d" % (i, c), [128, TOK], BF16) for c in range(2)] for i in range(2)]
        vb = [sc.sb("vb%d" % i, [128, NT, 132], BF16) for i in range(2)]
        NSP = 5
        pT = [sc.sb("pT%d" % i, [128, 512], BF16) for i in range(NSP)]
        osb = [[sc.sb("osb%d_%d" % (c, j), [128, 132], F32) for j in range(4)] for c in range(2)]
        oat = [sc.sb("oat%d" % i, [128, TOK], BF16) for i in range(2)]
        dl, Bdl = sc.sb("dl", [128, 256], F32)
        lamt, Blamt = sc.sb("lamt", [128, 8], F32)
        prod, Bprod = sc.sb("prod", [128, 128], F32)
        gsbc, Bgsbc = sc.sb("gsbc", [128, 128], F32)
        smalls = [sc.sb("sm%d" % i, [128, 8], F32) for i in range(4)]
        obuf = [sc.sb("ob%d" % i, [128, 128], F32) for i in range(4)]
        tbuf = [sc.sb("tb%d" % i, [128, 128], F32) for i in range(4)]
        onb = [sc.sb("onb%d" % i, [128, 128], BF16) for i in range(4)]
        junkc, Bjunkc = sc.sb("junkc", [128, 128], F32)
        ssq, Bssq = sc.sb("ssq", [128, 12], F32)
        SP = [sc.ps("SP%d" % i, [128, 512], F32) for i in range(NSP)]
        ACCB = [sc.ps("ACC%d" % i, [128, 512], F32) for i in range(2)]
        ACC = [(ACCB[j // 2][0], ACCB[j // 2][1], (j % 2) * 256) for j in range(4)]
        PTc, BPTc = sc.ps("PTc", [128, D], BF16)
        BPTcs = [Buf("PTc%d" % i) for i in range(8)]
        if MOE_SPARSE:
            NBz = ((NT - (2 if last else 0)) + 3) // 4 + 16
            z16, Bz16 = sc.sb("z16", [128, 4096], BF16)
            S.op("pool", lambda: nc.gpsimd.memset(z16[:], 0.0), writes=[Bz16])
            XSv = XS[0:NBz * BSL, :].rearrange("(p a) d -> p (a d)", p=128)
            for a_ in range(2 * NBz):
                S.dma("sp", XSv[:, a_ * 4096:(a_ + 1) * 4096], z16[:], reads=[Bz16], writes=[BXS])
        S.dma("sp", dl[:], dlam_d[li].partition_broadcast(128), writes=[Bdl])
        S.dma("sp", gsbc[:], gsub_d[li].partition_broadcast(128), writes=[Bgsbc])
        S.op("act", lambda: nc.scalar.mul(out=gsbc[:], in_=gsbc[:], mul=float(1.0 - lam_init)), reads=[Bgsbc], writes=[Bgsbc])
        S.op("dve", lambda: nc.vector.tensor_tensor(out=prod[:, 0:64], in0=dl[:, 0:64], in1=dl[:, 64:128], op=ALU.mult),
             reads=[Bdl], writes=[Bprod])
        S.op("dve", lambda: nc.vector.tensor_tensor(out=prod[:, 64:128], in0=dl[:, 128:192], in1=dl[:, 192:256], op=ALU.mult),
             reads=[Bdl], writes=[Bprod])
        S.op("dve", lambda: nc.vector.tensor_reduce(out=lamt[:, 0:1], in_=prod[:, 0:64], axis=AX.X, op=ALU.add),
             reads=[Bprod], writes=[Blamt])
        S.op("dve", lambda: nc.vector.tensor_reduce(out=lamt[:, 1:2], in_=prod[:, 64:128], axis=AX.X, op=ALU.add),
             reads=[Bprod], writes=[Blamt])
        S.op("act", lambda: nc.scalar.activation(out=lamt[:, 2:4], in_=lamt[:, 0:2], func=AF.Exp), reads=[Blamt], writes=[Blamt])
        S.op("dve", lambda: nc.vector.tensor_tensor(out=lamt[:, 4:5], in0=lamt[:, 3:4], in1=lamt[:, 2:3], op=ALU.subtract),
             reads=[Blamt], writes=[Blamt])
        S.op("dve", lambda: nc.vector.tensor_scalar(out=lamt[:, 5:6], in0=lamt[:, 4:5], scalar1=float(-lam_init), scalar2=None,
                                                     op0=ALU.add), reads=[Blamt], writes=[Blamt])
        for i in range(2):
            S.op("dve", lambda i=i: nc.vector.memset(vb[i][0][:, :, 128:132], 1.0), writes=[vb[i][1]])
            for c in range(2):
                o = 64 * (1 - c)
                S.op("pool", lambda i=i, c=c, o=o: nc.gpsimd.memset(qtb[i][c][0][o:o + 64, :], 0.0), writes=[qtb[i][c][1]])
        KTsrc = KT
        qchunks = []
        if not last:
            qchunks.append((0, LC, [0, 1]))
        for i in range(SEQ // 512):
            qchunks.append((LC + i * 512, 512, list(range(NT))))
        vsrc = VV.rearrange("(kt p) c -> p kt c", p=128)
        a0 = 0 if not last else 0
        for h in range(DBG.get("heads", 8)):
            kt_, Bkt = ktb[h % 2]
            qz = qtb[h % 2]
            v_, Bv = vb[h % 2]
            oa_, Boa = oat[h % 2]
            S.dma("sp", kt_[:], KT[h], reads=[BKT], writes=[Bkt])
            q0a = 0 if not last else LC
            for c in range(2):
                S.dma("sp", qz[c][0][c * 64:(c + 1) * 64, q0a:TOK], QT[h, c * 64:(c + 1) * 64, q0a:TOK], reads=[BQT], writes=[qz[c][1]])
            for g4 in range(0, NT, 6):
                g5 = min(NT, g4 + 6)
                S.dma("sp", v_[:, g4:g5, 0:128], vsrc[:, g4:g5, h * 128:(h + 1) * 128],
                      reads=[BVV], writes=[Bv])
            units = []
            for (q0, nq, kts) in qchunks[:DBG.get("nqch", 99)]:
                for c in range(2):
                    for ki, kt_i in enumerate(kts):
                        units.append((q0, nq, c, kt_i, ki == 0, ki == len(kts) - 1))

            def emit_qk(ui):
                q0, nq, c, kt_i, first, lastk = units[ui]
                sp_, Bsp = SP[ui % NSP]
                S.op("pe", lambda: nc.tensor.matmul(
                    sp_[:, 0:nq], lhsT=kt_[:, kt_i * 128:(kt_i + 1) * 128],
                    rhs=qz[c][0][:, q0:q0 + nq], start=True, stop=True),
                    reads=[Bkt, qz[c][1]], writes=[Bsp], sig=True)

            for u0 in range(min(NSP - 1, len(units))):
                emit_qk(u0)
            smi = 0
            deferred = []
            for ui, (q0, nq, c, kt_i, first, lastk) in enumerate(units):
                while deferred and deferred[0][0] <= ui:
                    deferred.pop(0)[1]()
                sp_, Bsp = SP[ui % NSP]
                p_, Bp = pT[ui % NSP]
                S.op("act", lambda: nc.scalar.activation(out=p_[:, 0:nq], in_=sp_[:, 0:nq], func=AF.Exp, scale=0.125),
                     reads=[Bsp], writes=[Bp])
                nj = nq // 128
                for j in range(nj):
                    ac, Bac, co = ACC[j]
                    S.op("pe", lambda j=j, ac=ac, co=co: nc.tensor.matmul(
                        ac[:, co:co + 130], lhsT=p_[:, j * 128:(j + 1) * 128], rhs=v_[:, kt_i, 0:130],
                        start=(first and j % 2 == 0), stop=lastk, skip_group_check=True),
                        reads=[Bp, Bv], writes=[Bac], sig=(j == nj - 1))
                if ui + NSP - 1 < len(units):
                    emit_qk(ui + NSP - 1)
                if lastk:
                    for j in range(nj):
                        ac, Bac, co = ACC[j]
                        ot, Bot = osb[c][j]
                        S.op("dve", lambda ac=ac, ot=ot, co=co: nc.vector.tensor_copy(out=ot[:, 0:130], in_=ac[:, co:co + 130]),
                             reads=[Bac], writes=[Bot])
                    if c == 1:
                        def stage1(nj=nj, q0=q0):
                            for j in range(nj):
                                o1, Bo1 = osb[0][j]
                                o2, Bo2 = osb[1][j]
                                sm, Bsm = smalls[j]
                                ob, Bob = obuf[j]
                                tb, Btb = tbuf[j]
                                S.op("dve", lambda: nc.vector.reciprocal(out=sm[:, 0:1], in_=o1[:, 128:129]), reads=[Bo1], writes=[Bsm])
                                S.op("dve", lambda: nc.vector.reciprocal(out=sm[:, 1:2], in_=o2[:, 128:129]), reads=[Bo2], writes=[Bsm])
                                S.op("dve", lambda: nc.vector.tensor_tensor(out=sm[:, 2:3], in0=sm[:, 1:2], in1=lamt[:, 5:6], op=ALU.mult),
                                     reads=[Bsm, Blamt], writes=[Bsm])
                                S.op("dve", lambda: nc.vector.tensor_scalar(out=tb[:], in0=o1[:, 0:128], scalar1=sm[:, 0:1], scalar2=None,
                                                                             op0=ALU.mult), reads=[Bo1, Bsm], writes=[Btb])
                                S.op("dve", lambda: nc.vector.scalar_tensor_tensor(out=ob[:], in0=o2[:, 0:128], scalar=sm[:, 2:3], in1=tb[:],
                                                                                    op0=ALU.mult, op1=ALU.add),
                                     reads=[Bo2, Bsm, Btb], writes=[Bob])
                                S.op("dve", lambda: nc.vector.tensor_tensor(out=tb[:], in0=ob[:], in1=ob[:], op=ALU.mult),
                                     reads=[Bob], writes=[Btb])
                                S.op("dve", lambda j=j: nc.vector.tensor_reduce(out=ssq[:, j:j + 1], in_=tb[:], axis=AX.X, op=ALU.add),
                                     reads=[Btb], writes=[Bssq])

                        def stage2(nj=nj):
                            S.op("act", lambda: nc.scalar.activation(out=ssq[:, 4:4 + nj], in_=ssq[:, 0:nj], func=AF.Ln, scale=1.0 / 128, bias=EPS),
                                 reads=[Bssq], writes=[Bssq])
                            S.op("act", lambda: nc.scalar.activation(out=ssq[:, 8:8 + nj], in_=ssq[:, 4:4 + nj], func=AF.Exp, scale=-0.5),
                                 reads=[Bssq], writes=[Bssq])

                        def stage3(nj=nj):
                            for j in range(nj):
                                ob, Bob = obuf[j]
                                on, Bon = onb[j]
                                S.op("dve", lambda j=j: nc.vector.scalar_tensor_tensor(out=on[:], in0=ob[:], scalar=ssq[:, 8 + j:9 + j], in1=gsbc[:],
                                                                                        op0=ALU.mult, op1=ALU.mult),
                                     reads=[Bob, Bssq, Bgsbc], writes=[Bon])

                        def stage4(nj=nj):
                            for j in range(nj):
                                on, Bon = onb[j]
                                S.op("pe", lambda j=j: nc.tensor.transpose(out=PTc[:, j * 128:(j + 1) * 128], in_=on[:], identity=identb[:]),
                                     reads=[Bon, Bident], writes=[BPTc], sig=(j == nj - 1))

                        def stage5(nj=nj, q0=q0):
                            S.op("dve", lambda: nc.vector.tensor_copy(out=oa_[:, q0:q0 + nj * 128], in_=PTc[:, 0:nj * 128]),
                                 reads=[BPTc], writes=[Boa])
                        for dly, fn_ in ((1, stage1), (18, stage2), (24, stage3), (30, stage4), (34, stage5)):
                            deferred.append((ui + dly, fn_))
                        deferred.sort(key=lambda x: x[0])
            for (_, fin) in deferred:
                fin()
            S.dma("sp", OAT[h, :, q0a:TOK], oa_[:, q0a:TOK], reads=[Boa], writes=[BOAT])
        S.barrier()
        sc.close()
        if stop_here("C%d" % li):
            done = True
            break

        sc = Scope(nc, "D%d" % li)
        rxb, Brx = sc.sb("rxb", [128, TOK], F32)
        grgb = [sc.sb("grgb%d" % i, [128, TOK], BF16) for i in range(2)]
        ub, Bu = sc.sb("ub", [128, TOK], F32)
        u16, Bu16 = sc.sb("u16", [128, TOK], BF16)
        ra, Bra = sc.sb("ra", [128, TOK], F32)
        ib, Bib = sc.sb("ib", [128, TOK], F32)
        tt_, Btt = sc.sb("ttd", [128, TOK], F32)
        hf, Bhf = sc.sb("hf", [128, TOK], F32)
        hb, Bhb = sc.sb("hb", [128, TOK], F32)
        zst = [sc.sb("zst%d" % i, [128, TOK], BF16) for i in range(2)]
        cwt, Bcw = sc.sb("cwt", [128, 8, 4], F32)
        cbt, Bcb = sc.sb("cbt", [128, 8], F32)
        lwa, Blwa = sc.sb("lwa", [128, 16 * 128], BF16)
        lwx, Blwx = sc.sb("lwx", [128, 16 * 128], BF16)
        lba, Blba = sc.sb("lba", [128, 16], F32)
        lbx, Blbx = sc.sb("lbx", [128, 16], F32)
        asc, Basc = sc.sb("asc", [128, 16], F32)
        PD = [sc.ps("PD%d" % i, [128, 512], F32) for i in range(6)]
        S.dma("sp", cwt[:], cw_d[li], writes=[Bcw])
        S.dma("sp", cbt[:], cb_d[li], writes=[Bcb])
        S.dma("pool", lwa[:], lwa_d[li], writes=[Blwa])
        S.dma("pool", lwx[:], lwx_d[li], writes=[Blwx])
        S.dma("sp", lba[:], lba_d[li], writes=[Blba])
        S.dma("sp", lbx[:], lbx_d[li], writes=[Blbx])
        S.dma("sp", asc[:], llam_d[li], writes=[Basc])
        S.op("act", lambda: nc.scalar.activation(out=asc[:], in_=asc[:], func=AF.Exp, scale=-1.0), reads=[Basc], writes=[Basc])
        S.op("act", lambda: nc.scalar.activation(out=asc[:], in_=asc[:], func=AF.Ln, bias=1.0), reads=[Basc], writes=[Basc])
        S.op("dve", lambda: nc.vector.tensor_scalar(out=asc[:], in0=asc[:], scalar1=-8.0, scalar2=None, op0=ALU.mult),
             reads=[Basc], writes=[Basc])
        NLS = 8
        SGL = SEQ // NLS
        SEGS = [(0, LC, 0, LC)] + [(LC + i * SGL, LC + (i + 1) * SGL, LC, TOK) for i in range(NLS)]
        NSG = len(SEGS)
        pdi = 0
        for n in range(8):
            gg, Bgg = grgb[n % 2]
            zz, Bzz = zst[n % 2]
            S.dma("sp", rxb[:], RX[n], reads=[BRX], writes=[Brx])
            g0 = 0 if not last else LC
            S.dma("sp", gg[:, g0:TOK], GRG[n, :, g0:TOK], reads=[BGRG], writes=[Bgg])
            Bu_s = [Buf("u%d" % i) for i in range(NSG)]
            Bu16_s = [Buf("u16_%d" % i) for i in range(NSG)]
            Bra_s = [Buf("ra%d" % i) for i in range(NSG)]
            Bib_s = [Buf("ib%d" % i) for i in range(NSG)]
            Btt_s = [Buf("tt%d" % i) for i in range(NSG)]
            Bhf_s = [Buf("hf%d" % i) for i in range(NSG)]
            Bhb_s = [Buf("hb%d" % i) for i in range(NSG)]
            for B_, Bs in ((Bu, Bu_s), (Bu16, Bu16_s), (Bra, Bra_s), (Bib, Bib_s), (Btt, Btt_s), (Bhf, Bhf_s), (Bhb, Bhb_s)):
                for b_ in Bs:
                    b_.writer = B_.writer
                    b_.readers = list(B_.readers)
            for si, (s0, s1, q0_, q1_) in enumerate(SEGS):
                S.op("dve", lambda n=n, s0=s0, s1=s1: nc.vector.tensor_scalar(
                    out=ub[:, s0:s1], in0=rxb[:, s0:s1], scalar1=cwt[:, n, 2:3], scalar2=cbt[:, n:n + 1],
                    op0=ALU.mult, op1=ALU.add), reads=[Brx, Bcw, Bcb], writes=[Bu_s[si]])
                for (j, off) in ((0, -2), (1, -1), (3, 1)):
                    o0 = max(s0, q0_ - off)
                    o1 = min(s1, q1_ - off)
                    S.op("dve", lambda n=n, j=j, o0=o0, o1=o1, off=off: nc.vector.scalar_tensor_tensor(
                        out=ub[:, o0:o1], in0=rxb[:, o0 + off:o1 + off], scalar=cwt[:, n, j:j + 1],
                        in1=ub[:, o0:o1], op0=ALU.mult, op1=ALU.add), reads=[Brx, Bcw, Bu_s[si]], writes=[Bu_s[si]])
                S.op("pool", lambda s0=s0, s1=s1: nc.gpsimd.tensor_copy(out=u16[:, s0:s1], in_=ub[:, s0:s1]),
                     reads=[Bu_s[si]], writes=[Bu16_s[si]])
            for dr in range(2):
                wi = dr * 8 + n
                order = list(range(NSG)) if dr == 0 else [0] + list(range(NSG - 1, 0, -1))

                def st_gate(si, which):
                    global_pdi = None
                    s0, s1 = SEGS[si][0], SEGS[si][1]
                    wt, bt, dstb, Bd, Bwt, Bbt = (lwa, lba, ra, Bra_s[si], Blwa, Blba) if which == 0 else (lwx, lbx, ib, Bib_s[si], Blwx, Blbx)
                    for c0 in range(s0, s1, 512):
                        nn = min(512, s1 - c0)
                        pd_, Bpd = PD[st_gate.pdi % 6]
                        st_gate.pdi += 1
                        S.op("pe", lambda: nc.tensor.matmul(
                            pd_[:, 0:nn], lhsT=wt[:, wi * 128:(wi + 1) * 128], rhs=u16[:, c0:c0 + nn], start=True, stop=True),
                            reads=[Bwt, Bu16_s[si]], writes=[Bpd], sig=True)
                        S.op("act", lambda: nc.scalar.activation(
                            out=dstb[:, c0:c0 + nn], in_=pd_[:, 0:nn], func=AF.Sigmoid, bias=bt[:, wi:wi + 1]),
                            reads=[Bpd, Bbt], writes=[Bd])
                st_gate.pdi = pdi

                def stage(k, oi):
                    si = order[oi]
                    s0, s1 = SEGS[si][0], SEGS[si][1]
                    if k == 0:
                        st_gate(si, 0)
                    elif k == 1:
                        st_gate(si, 1)
                    elif k == 2:
                        S.op("act", lambda: nc.scalar.activation(out=ra[:, s0:s1], in_=ra[:, s0:s1], func=AF.Exp, scale=asc[:, wi:wi + 1]),
                             reads=[Bra_s[si], Basc], writes=[Bra_s[si]])
                    elif k == 3:
                        S.op("dve", lambda: nc.vector.scalar_tensor_tensor(out=tt_[:, s0:s1], in0=ra[:, s0:s1], scalar=-1.0, in1=ra[:, s0:s1],
                                                                            op0=ALU.mult, op1=ALU.mult), reads=[Bra_s[si]], writes=[Btt_s[si]])
                    elif k == 4:
                        S.op("act", lambda: nc.scalar.activation(out=tt_[:, s0:s1], in_=tt_[:, s0:s1], func=AF.Sqrt, bias=1.0),
                             reads=[Btt_s[si]], writes=[Btt_s[si]])
                    elif k == 5:
                        S.op("pool", lambda: nc.gpsimd.tensor_tensor(out=ib[:, s0:s1], in0=ib[:, s0:s1], in1=tt_[:, s0:s1], op=ALU.mult),
                             reads=[Bib_s[si], Btt_s[si]], writes=[Bib_s[si]])
                        S.op("pool", lambda: nc.gpsimd.tensor_tensor(out=ib[:, s0:s1], in0=ib[:, s0:s1], in1=ub[:, s0:s1], op=ALU.mult),
                             reads=[Bib_s[si], Bu_s[si]], writes=[Bib_s[si]])
                    elif k == 6:
                        if dr == 0:
                            if oi == 0:
                                init, rd = 0.0, []
                            else:
                                init, rd = hf[:, s0 - 1:s0], [Bhf_s[order[oi - 1]]]
                            S.op("dve", lambda: nc.vector.tensor_tensor_scan(out=hf[:, s0:s1], data0=ra[:, s0:s1], data1=ib[:, s0:s1], initial=init,
                                                                              op0=ALU.mult, op1=ALU.add),
                                 reads=[Bra_s[si], Bib_s[si]] + rd, writes=[Bhf_s[si]])
                        else:
                            if oi == 0:
                                init, rd = 0.0, []
                            elif oi == 1:
                                init, rd = hb[:, 0:1], [Bhb_s[0]]
                            else:
                                ps0 = SEGS[order[oi - 1]][0]
                                init, rd = hb[:, ps0:ps0 + 1], [Bhb_s[order[oi - 1]]]
                            S.op("dve", lambda: nc.vector.tensor_tensor_scan(out=hb[:, s0:s1][:, ::-1], data0=ra[:, s0:s1][:, ::-1],
                                                                              data1=ib[:, s0:s1][:, ::-1], initial=init,
                                                                              op0=ALU.mult, op1=ALU.add),
                                 reads=[Bra_s[si], Bib_s[si]] + rd, writes=[Bhb_s[si]])
                NST = 7
                for w in range(NSG + NST - 1):
                    for k in range(NST):
                        oi = w - k
                        if 0 <= oi < NSG:
                            stage(k, oi)
                pdi = st_gate.pdi
            for si, (s0, s1, _, _) in enumerate(SEGS):
                if last and si == 0:
                    continue
                S.op("pool", lambda s0=s0, s1=s1: nc.gpsimd.tensor_tensor(out=hf[:, s0:s1], in0=hf[:, s0:s1], in1=hb[:, s0:s1], op=ALU.add),
                     reads=[Bhf_s[si], Bhb_s[si]], writes=[Bhf_s[si]])
                S.op("pool", lambda s0=s0, s1=s1, gg=gg, zz=zz: nc.gpsimd.tensor_tensor(out=zz[:, s0:s1], in0=hf[:, s0:s1], in1=gg[:, s0:s1], op=ALU.mult),
                     reads=[Bhf_s[si], Bgg], writes=[Bzz])
            for B_, Bs in ((Bu, Bu_s), (Bu16, Bu16_s), (Bra, Bra_s), (Bib, Bib_s), (Btt, Btt_s), (Bhf, Bhf_s), (Bhb, Bhb_s)):
                B_.writer = None
                B_.readers = []
                for b_ in Bs:
                    if b_.writer is not None:
                        B_.readers.append(b_.writer)
                    B_.readers.extend(b_.readers)
            S.dma("sp", ZT[n, :, g0:TOK], zz[:, g0:TOK], reads=[Bzz], writes=[BZT])
        S.barrier()
        sc.close()
        if stop_here("D%d" % li):
            done = True
            break

        sc = Scope(nc, "E%d" % li)
        wr, Bwr = sc.sb("wr", [128, 8, D], BF16)
        wa, Bwa = sc.sb("wa", [128, 8, D], BF16)
        wo, Bwo = sc.sb("wo", [128, 8, D], BF16)
        ztb = [sc.sb("ztb%d" % i, [128, 8, 512], BF16) for i in range(2)]
        oab = [sc.sb("oab%d" % i, [128, 8, 512], BF16) for i in range(2)]
        srb = [sc.sb("srb%d" % i, [128, 8, 512], BF16) for i in range(2)]
        sab = [sc.sb("sab%d" % i, [128, 8, 512], BF16) for i in range(2)]
        xin = [sc.sb("xin%d" % i, [128, 4, D], F32) for i in range(1)]
        mT, BmT = sc.sb("mT", [128, 8, 512], BF16)
        tA = [sc.sb("tA%d" % i, [128, 512], F32) for i in range(2)]
        tB = [sc.sb("tB%d" % i, [128, 512], F32) for i in range(2)]
        tC = [sc.sb("tC%d" % i, [128, 512], F32) for i in range(2)]
        x1 = [sc.sb("x1_%d" % i, [128, D], F32) for i in range(2)]
        tmpsE = [(sc.sb("junkE%d" % i, [128, D], BF16) + sc.sb("ssE%d" % i, [128, 4], F32) + sc.sb("xnE%d" % i, [128, D], BF16))
                 for i in range(2)]
        h2st = [sc.sb("h2st%d" % i, [128, 8, 512], BF16) for i in range(2)]
        h2tk = [sc.sb("h2tk%d" % i, [128, D], BF16) for i in range(2)]

        PE_ = [sc.ps("PE%d" % i, [128, 512], F32) for i in range(6)]
        PTe = [sc.ps("PTe%d" % i, [128, D], BF16) for i in range(2)]
        for (wt, Bw, src) in ((wr, Bwr, wrp_d), (wa, Bwa, wap_d), (wo, Bwo, wo_d)):
            for hh in range(2):
                S.dma("pool", wt[:, hh * 4:(hh + 1) * 4, :], src[li].rearrange("(kc p) n -> p kc n", p=128)[:, hh * 4:(hh + 1) * 4, :],
                      writes=[Bw])
        pei = 0
        tci = 0
        for ci, (tok0, n, isctx) in enumerate(chunks):
            jj = 1 if isctx else 0
            zt_, Bzt = ztb[ci % 2]
            oa_, Boa = oab[ci % 2]
            sr_, Bsr = srb[ci % 2]
            sa_, Bsa = sab[ci % 2]
            xi_, Bxi = xin[0]
            h2_, Bh2 = h2st[ci % 2]
            S.dma("sp", zt_[:, :, 0:n], ZT.rearrange("c p t -> p c t")[:, :, tok0:tok0 + n], reads=[BZT], writes=[Bzt])
            S.dma("sp", oa_[:, :, 0:n], OAT.rearrange("c p t -> p c t")[:, :, tok0:tok0 + n], reads=[BOAT], writes=[Boa])
            S.dma("sp", sr_[:, :, 0:n], SGR.rearrange("c p t -> p c t")[:, :, tok0:tok0 + n], reads=[BSGR], writes=[Bsr])
            S.dma("sp", sa_[:, :, 0:n], SGA.rearrange("c p t -> p c t")[:, :, tok0:tok0 + n], reads=[BSGA], writes=[Bsa])
            nt_ = n // 128
            S.dma("sp", xi_[:, 0:nt_, :], xcur[tok0:tok0 + n, :].rearrange("(j p) d -> p j d", p=128), reads=[Bxcur], writes=[Bxi])
            for dc in range(8):
                por, Bpor = PE_[pei % 6]
                pei += 1
                poa, Bpoa = PE_[pei % 6]
                pei += 1
                for kc in range(8):
                    S.op("pe", lambda kc=kc, dc=dc, por=por: nc.tensor.matmul(
                        por[:, 0:n], lhsT=wr[:, kc, dc * 128:(dc + 1) * 128], rhs=zt_[:, kc, 0:n], start=(kc == 0), stop=(kc == 7)),
                        reads=[Bwr, Bzt], writes=[Bpor], sig=(kc == 7))
                for kc in range(8):
                    S.op("pe", lambda kc=kc, dc=dc, poa=poa: nc.tensor.matmul(
                        poa[:, 0:n], lhsT=wa[:, kc, dc * 128:(dc + 1) * 128], rhs=oa_[:, kc, 0:n], start=(kc == 0), stop=(kc == 7)),
                        reads=[Bwa, Boa], writes=[Bpoa], sig=(kc == 7))
                ta, Bta = tA[dc % 2]
                tb_, Btb_ = tB[dc % 2]
                S.op("dve", lambda dc=dc, ta=ta, por=por: nc.vector.tensor_tensor(out=ta[:, 0:n], in0=por[:, 0:n], in1=sr_[:, dc, 0:n], op=ALU.mult),
                     reads=[Bpor, Bsr], writes=[Bta])
                S.op("dve", lambda dc=dc, tb_=tb_, poa=poa: nc.vector.tensor_tensor(out=tb_[:, 0:n], in0=poa[:, 0:n], in1=sa_[:, dc, 0:n], op=ALU.mult),
                     reads=[Bpoa, Bsa], writes=[Btb_])
                S.op("pool", lambda dc=dc, ta=ta, tb_=tb_: nc.gpsimd.tensor_tensor(out=mT[:, dc, 0:n], in0=ta[:, 0:n], in1=tb_[:, 0:n], op=ALU.add),
                     reads=[Bta, Btb_], writes=[BmT])
            for j in range(nt_):
                tile_i = tok0 // 128 + j
                x1_, Bx1 = x1[tile_i % 2]
                for half in range(2):
                    po, Bpo = PE_[pei % 6]
                    pei += 1
                    for kc in range(8):
                        S.op("pe", lambda kc=kc, j=j, half=half, po=po: nc.tensor.matmul(
                            po[:, :], lhsT=mT[:, kc, j * 128:(j + 1) * 128], rhs=wo[:, kc, half * 512:(half + 1) * 512],
                            start=(kc == 0), stop=(kc == 7)), reads=[BmT, Bwo], writes=[Bpo], sig=(kc == 7))
                    tc_, Btc = tC[tci % 2]
                    tci += 1
                    S.op("dve", lambda half=half, po=po, tc_=tc_: nc.vector.tensor_tensor(
                        out=tc_[:], in0=po[:, :], in1=gt1bc[:, jj, half * 512:(half + 1) * 512], op=ALU.mult),
                        reads=[Bpo, Bgt1], writes=[Btc])
                    S.op("pool", lambda half=half, j=j, tc_=tc_, x1_=x1_: nc.gpsimd.tensor_tensor(
                        out=x1_[:, half * 512:(half + 1) * 512], in0=tc_[:], in1=xi_[:, j, half * 512:(half + 1) * 512], op=ALU.add),
                        reads=[Btc, Bxi], writes=[Bx1])
                S.dma("sp", XMID[tile_i * 128:(tile_i + 1) * 128, :], x1_[:], reads=[Bx1], writes=[BXMID])
                PT, BPT = PTe[tile_i % 2]
                rmsnorm_T(sc, x1_[:], Bx1, li, 1, jj, lambda kc, j=j: h2_[:, kc, j * 128:(j + 1) * 128], Bh2, PT, BPT, tmpsE[tile_i % 2])
                pr, Bpr = PE_[pei % 6]
                pei += 1
                for kc in range(8):
                    S.op("pe", lambda kc=kc, j=j, pr=pr: nc.tensor.matmul(
                        pr[:, 0:NE], lhsT=h2_[:, kc, j * 128:(j + 1) * 128], rhs=wrt[:, kc, :], start=(kc == 0), stop=(kc == 7)),
                        reads=[Bh2, Bwrt], writes=[Bpr], sig=(kc == 7))
                S.op("dve", lambda pr=pr, tile_i=tile_i: nc.vector.tensor_tensor(out=logits[:, tile_i, :], in0=pr[:, 0:NE], in1=brtbc[:], op=ALU.add),
                     reads=[Bpr, Bbrt], writes=[Blog])
                if MOE_SPARSE:
                    PT2, BPT2 = PTe[(tile_i + 1) % 2]
                    for kc in range(8):
                        S.op("pe", lambda kc=kc, j=j, PT2=PT2: nc.tensor.transpose(
                            out=PT2[:, kc * 128:(kc + 1) * 128], in_=h2_[:, kc, j * 128:(j + 1) * 128], identity=identb[:]),
                            reads=[Bh2, Bident], writes=[BPT2], sig=(kc == 7))
                    h2k, Bh2k = h2tk[tile_i % 2]
                    S.op("dve", lambda PT2=PT2, h2k=h2k: nc.vector.tensor_copy(out=h2k[:], in_=PT2[:, :]), reads=[BPT2], writes=[Bh2k])
                    S.dma("sp", H2TOK[tile_i * 128:(tile_i + 1) * 128, :], h2k[:], reads=[Bh2k], writes=[BH2TOK])
            S.dma("sp", H2T[:, :, tok0:tok0 + n], h2_[:, :, 0:n], reads=[Bh2], writes=[BH2T])
        S.barrier()
        sc.close()
        if stop_here("E%d" % li):
            done = True
            break

        t_lo = 2 if last else 0
        ntl = NT - t_lo
        sc = Scope(nc, "F%d" % li)
        if MOE_SPARSE:
            d1i, Bd1i = sc.sb("d1i", [128, NT], I32)
            d2i, Bd2i = sc.sb("d2i", [128, NT], I32)
            g12, Bg12 = sc.sb("g12", [128, 2, NT], F32)
            idxw, Bidxw = sc.sb("idxw", [128, NBMAX, 8], I32)
        scR = Scope(nc, "FR%d" % li)
        r_gm, Bgm = scR.sb("r_gm", [128, NT, 4], F32)
        r_gx, Bgx = scR.sb("r_gx", [128, NT], F32)
        r_oh, Boh = scR.sb("r_oh", [128, NT, 4], F32)
        r_ml, Bml = scR.sb("r_ml", [128, NT, NE], F32)
        r_e1, Be1 = scR.sb("r_e1", [128, NT, NE], F32)
        r_m2, Bm2 = scR.sb("r_m2", [128, NT, NE], F32)
        r_x2, Bx2 = scR.sb("r_x2", [128, NT], F32)
        r_e2, Be2 = scR.sb("r_e2", [128, NT, NE], F32)
        r_d, Bd_ = scR.sb("r_d", [128, 4, NT], F32)
        sl = slice(t_lo, NT)
        L4 = logits[:, sl, :].rearrange("p t (g e) -> p t g e", g=4)
        S.op("dve", lambda: nc.vector.tensor_reduce(out=r_gm[:, sl, :], in_=L4, axis=AX.X, op=ALU.max), reads=[Blog], writes=[Bgm])
        S.op("dve", lambda: nc.vector.tensor_reduce(out=r_gx[:, sl], in_=r_gm[:, sl, :], axis=AX.X, op=ALU.max), reads=[Bgm], writes=[Bgx])
        S.op("dve", lambda: nc.vector.tensor_tensor(out=r_oh[:, sl, :], in0=r_gm[:, sl, :],
                                                     in1=r_gx[:, sl].unsqueeze(2).to_broadcast([128, ntl, 4]), op=ALU.is_equal),
             reads=[Bgm, Bgx], writes=[Boh])
        S.op("dve", lambda: nc.vector.tensor_scalar(out=r_oh[:, sl, :], in0=r_oh[:, sl, :], scalar1=-1.0, scalar2=1e30,
                                                     op0=ALU.add, op1=ALU.mult), reads=[Boh], writes=[Boh])
        S.op("dve", lambda: nc.vector.tensor_tensor(out=r_ml[:, sl, :].rearrange("p t (g e) -> p t g e", g=4), in0=L4,
                                                     in1=r_oh[:, sl, :].unsqueeze(3).to_broadcast([128, ntl, 4, 4]), op=ALU.add),
             reads=[Blog, Boh], writes=[Bml])
        S.op("dve", lambda: nc.vector.tensor_tensor(out=r_e1[:, sl, :], in0=r_ml[:, sl, :],
                                                     in1=r_gx[:, sl].unsqueeze(2).to_broadcast([128, ntl, NE]), op=ALU.is_equal),
             reads=[Bml, Bgx], writes=[Be1])
        S.op("dve", lambda: nc.vector.scalar_tensor_tensor(out=r_m2[:, sl, :], in0=r_e1[:, sl, :], scalar=-1e30, in1=r_ml[:, sl, :],
                                                            op0=ALU.mult, op1=ALU.add), reads=[Be1, Bml], writes=[Bm2])
        S.op("dve", lambda: nc.vector.tensor_reduce(out=r_x2[:, sl], in_=r_m2[:, sl, :], axis=AX.X, op=ALU.max), reads=[Bm2], writes=[Bx2])
        S.op("dve", lambda: nc.vector.tensor_tensor(out=r_e2[:, sl, :], in0=r_m2[:, sl, :],
                                                     in1=r_x2[:, sl].unsqueeze(2).to_broadcast([128, ntl, NE]), op=ALU.is_equal),
             reads=[Bm2, Bx2], writes=[Be2])
        S.op("dve", lambda: nc.vector.tensor_tensor(out=r_d[:, 0, sl], in0=r_x2[:, sl], in1=r_gx[:, sl], op=ALU.subtract),
             reads=[Bx2, Bgx], writes=[Bd_])
        S.op("act", lambda: nc.scalar.activation(out=r_d[:, 1, sl], in_=r_d[:, 0, sl], func=AF.Exp), reads=[Bd_], writes=[Bd_])
        S.op("dve", lambda: nc.vector.tensor_scalar(out=r_d[:, 2, sl], in0=r_d[:, 1, sl], scalar1=1.0, scalar2=None, op0=ALU.add),
             reads=[Bd_], writes=[Bd_])
        S.op("dve", lambda: nc.vector.reciprocal(out=r_d[:, 2, sl], in_=r_d[:, 2, sl]), reads=[Bd_], writes=[Bd_])
        S.op("dve", lambda: nc.vector.tensor_tensor(out=r_d[:, 3, sl], in0=r_d[:, 1, sl], in1=r_d[:, 2, sl], op=ALU.mult),
             reads=[Bd_], writes=[Bd_])
        if MOE_SPARSE:
            NB = (ntl + 3) // 4 + 16
            mk, Bmk = scR.sb("mk", [128, NT, NE], F32)
            mk16, Bmk16 = scR.sb("mk16", [128, NT, NE], BF16)
            pre, Bpre = scR.sb("pre", [128, NT, NE], F32)
            tot, Btot = scR.sb("tot", [128, NT, NE], F32)
            cum, Bcum = scR.sb("cum", [128, NT, NE], F32)
            onesn, Bonesn = scR.sb("onesn", [128, 40], F32)
            sE, BsE = scR.sb("sE", [128, 8, NE], F32)
            cmpE, BcmpE = scR.sb("cmpE", [128, NE, 9], F32)
            cmpB, BcmpB = scR.sb("cmpB", [128, NBMAX, NE], F32)
            bef, Bbef = scR.sb("bef", [128, NBMAX], F32)
            idxf, Bidxf = scR.sb("idxf", [128, NBMAX, 8], F32)
            dsum, Bdsum = scR.sb("dsum", [128, 2, NT], F32)
            PRt = [scR.ps("PRt%d" % i, [128, 512], F32) for i in range(4)]
            S.op("dve", lambda: nc.vector.tensor_tensor(out=mk[:, sl, :], in0=r_e1[:, sl, :], in1=r_e2[:, sl, :], op=ALU.add),
                 reads=[Be1, Be2], writes=[Bmk])
            S.op("dve", lambda: nc.vector.tensor_copy(out=mk16[:, sl, :], in_=mk[:, sl, :]), reads=[Bmk], writes=[Bmk16])
            ncols = ntl * NE
            hc = ncols // 2
            mk16f = mk16[:, sl, :].rearrange("p t e -> p (t e)")
            pref = pre[:, sl, :].rearrange("p t e -> p (t e)")
            totf = tot[:, sl, :].rearrange("p t e -> p (t e)")
            for hh in range(2):
                c0 = hh * hc
                S.op("pe", lambda hh=hh, c0=c0: nc.tensor.matmul(PRt[hh][0][:, 0:hc], lhsT=utri16[:], rhs=mk16f[:, c0:c0 + hc], start=True, stop=True),
                     reads=[Butri, Bmk16], writes=[PRt[hh][1]], sig=True)
                S.op("pe", lambda hh=hh, c0=c0: nc.tensor.matmul(PRt[2 + hh][0][:, 0:hc], lhsT=ones16m[:], rhs=mk16f[:, c0:c0 + hc], start=True, stop=True),
                     reads=[Bonesm, Bmk16], writes=[PRt[2 + hh][1]], sig=True)
                S.op("dve", lambda hh=hh, c0=c0: nc.vector.tensor_copy(out=pref[:, c0:c0 + hc], in_=PRt[hh][0][:, 0:hc]), reads=[PRt[hh][1]], writes=[Bpre])
                S.op("act", lambda hh=hh, c0=c0: nc.scalar.copy(out=totf[:, c0:c0 + hc], in_=PRt[2 + hh][0][:, 0:hc]), reads=[PRt[2 + hh][1]], writes=[Btot])
            S.op("dve", lambda: nc.vector.memset(onesn[:], 1.0), writes=[Bonesn])
            for e in range(NE):
                S.op("dve", lambda e=e: nc.vector.tensor_tensor_scan(out=cum[:, sl, e], data0=onesn[:, 0:ntl], data1=tot[:, sl, e], initial=0.0,
                                                                      op0=ALU.mult, op1=ALU.add), reads=[Bonesn, Btot], writes=[Bcum])
            S.op("dve", lambda: nc.vector.tensor_copy(out=sE[:, 0, :], in_=cum[:, NT - 1, :]), reads=[Bcum], writes=[BsE])
            S.op("dve", lambda: nc.vector.tensor_tensor(out=tot[:, sl, :], in0=cum[:, sl, :], in1=tot[:, sl, :], op=ALU.subtract),
                 reads=[Bcum, Btot], writes=[Btot])
            S.op("dve", lambda: nc.vector.tensor_tensor(out=cmpE[:], in0=sE[:, 0, :].unsqueeze(2).to_broadcast([128, NE, 9]),
                                                         in1=thr[:, 0:18:2].unsqueeze(1).to_broadcast([128, NE, 9]), op=ALU.is_gt),
                 reads=[BsE, Bthr], writes=[BcmpE])
            S.op("dve", lambda: nc.vector.tensor_reduce(out=sE[:, 1, :], in_=cmpE[:], axis=AX.X, op=ALU.add), reads=[BcmpE], writes=[BsE])
            S.op("dve", lambda: nc.vector.tensor_scalar(out=sE[:, 2, :], in0=sE[:, 1, :], scalar1=float(BSL), scalar2=None, op0=ALU.mult),
                 reads=[BsE], writes=[BsE])
            S.op("dve", lambda: nc.vector.tensor_tensor_scan(out=sE[:, 3, :], data0=onesn[:, 0:NE], data1=sE[:, 2, :], initial=0.0,
                                                              op0=ALU.mult, op1=ALU.add), reads=[Bonesn, BsE], writes=[BsE])
            S.op("dve", lambda: nc.vector.tensor_tensor(out=sE[:, 4, :], in0=sE[:, 3, :], in1=sE[:, 2, :], op=ALU.subtract), reads=[BsE], writes=[BsE])
            S.op("dve", lambda: nc.vector.tensor_tensor(out=pre[:, sl, :], in0=pre[:, sl, :], in1=tot[:, sl, :], op=ALU.add),
                 reads=[Bpre, Btot], writes=[Bpre])
            S.op("dve", lambda: nc.vector.tensor_tensor(out=pre[:, sl, :], in0=pre[:, sl, :],
                                                         in1=sE[:, 4, :].unsqueeze(1).to_broadcast([128, ntl, NE]), op=ALU.add),
                 reads=[Bpre, BsE], writes=[Bpre])
            for k_, (re_, Bre_, di_, Bdi_) in enumerate(((r_e1, Be1, d1i, Bd1i), (r_e2, Be2, d2i, Bd2i))):
                S.op("dve", lambda re_=re_: nc.vector.tensor_tensor(out=mk[:, sl, :], in0=re_[:, sl, :], in1=pre[:, sl, :], op=ALU.mult),
                     reads=[Bre_, Bpre], writes=[Bmk])
                S.op("dve", lambda k_=k_: nc.vector.tensor_reduce(out=dsum[:, k_, sl], in_=mk[:, sl, :], axis=AX.X, op=ALU.add),
                     reads=[Bmk], writes=[Bdsum])
                S.op("dve", lambda k_=k_, di_=di_: nc.vector.tensor_copy(out=di_[:, sl], in_=dsum[:, k_, sl]), reads=[Bdsum], writes=[Bdi_])
            S.op("dve", lambda: nc.vector.tensor_copy(out=g12[:, :, sl], in_=r_d[:, 2:4, sl]), reads=[Bd_], writes=[Bg12])
            S.op("dve", lambda: nc.vector.tensor_tensor(out=cmpB[:, 0:NB, :], in0=sE[:, 3, :].unsqueeze(1).to_broadcast([128, NB, NE]),
                                                         in1=thr[:, 0:2 * NB:2].unsqueeze(2).to_broadcast([128, NB, NE]), op=ALU.is_le),
                 reads=[BsE, Bthr], writes=[BcmpB])
            S.op("dve", lambda: nc.vector.tensor_reduce(out=bef[:, 0:NB], in_=cmpB[:, 0:NB, :], axis=AX.X, op=ALU.add), reads=[BcmpB], writes=[Bbef])
            S.op("dve", lambda: nc.vector.tensor_scalar(out=bef[:, 0:NB], in0=bef[:, 0:NB], scalar1=15.0, scalar2=None, op0=ALU.min),
                 reads=[Bbef], writes=[Bbef])
            S.op("dve", lambda: nc.vector.scalar_tensor_tensor(out=idxf[:, 0:NB, :], in0=bef[:, 0:NB].unsqueeze(2).to_broadcast([128, NB, 8]), scalar=1024.0,
                                                                in1=prow[:].unsqueeze(1).to_broadcast([128, NB, 8]), op0=ALU.mult, op1=ALU.add),
                 reads=[Bbef, Bprow], writes=[Bidxf])
            S.op("dve", lambda: nc.vector.tensor_scalar(out=idxf[:, 0:NB, :], in0=idxf[:, 0:NB, :], scalar1=float(li * NE * D), scalar2=None, op0=ALU.add),
                 reads=[Bidxf], writes=[Bidxf])
            S.op("dve", lambda: nc.vector.tensor_copy(out=idxw[:, 0:NB, :], in_=idxf[:, 0:NB, :]), reads=[Bidxf], writes=[Bidxw])
        else:
            S.op("dve", lambda: nc.vector.tensor_tensor(out=r_e1[:, sl, :], in0=r_e1[:, sl, :],
                                                         in1=r_d[:, 2, sl].unsqueeze(2).to_broadcast([128, ntl, NE]), op=ALU.mult),
                 reads=[Be1, Bd_], writes=[Be1])
            S.op("dve", lambda: nc.vector.tensor_tensor(out=r_e2[:, sl, :], in0=r_e2[:, sl, :],
                                                         in1=r_d[:, 3, sl].unsqueeze(2).to_broadcast([128, ntl, NE]), op=ALU.mult),
                 reads=[Be2, Bd_], writes=[Be2])
            S.op("dve", lambda: nc.vector.tensor_tensor(out=gates[:, sl, :], in0=r_e1[:, sl, :], in1=r_e2[:, sl, :], op=ALU.add),
                 reads=[Be1, Be2], writes=[Bgates])
        if stop_here("R%d" % li):
            S.barrier()
            done = True
            break
        S.barrier()
        scR.close()
        if MOE_SPARSE:
            scS = Scope(nc, "FS%d" % li)
            h2a, Bh2a = scS.sb("h2a", [128, ntl, D], BF16)
            hsrc = H2TOK[t_lo * 128:TOK, :].rearrange("(t p) d -> p t d", p=128)
            for a_ in range(0, ntl, 8):
                b_ = min(ntl, a_ + 8)
                S.dma("sp", h2a[:, a_:b_, :], hsrc[:, a_:b_, :], reads=[BH2TOK], writes=[Bh2a])
            for t in range(t_lo, NT):
                for (di_, Bdi_) in ((d1i, Bd1i), (d2i, Bd2i)):
                    S.idma(out=XS, out_offset=bass.IndirectOffsetOnAxis(ap=di_[:, t:t + 1], axis=0), in_=h2a[:, t - t_lo, :], in_offset=None,
                           reads=[Bh2a, Bdi_], writes=[BXS])
            S.barrier()
            scS.close()
            w1b = [sc.sb("w1b%d" % i, [128, 8, D], BF16) for i in range(2)]
            w3b = [sc.sb("w3b%d" % i, [128, 8, D], BF16) for i in range(2)]
            w2b = [sc.sb("w2b%d" % i, [128, 8, D], BF16) for i in range(1)]
            xst = [sc.sb("xst%d" % i, [128, 4, D], BF16) for i in range(2)]
            xT, BxT = sc.sb("xT", [128, 8, 512], BF16)
            aT_, BaT = sc.sb("aTs", [128, 8, 512], BF16)
            tS = [sc.sb("tS%d" % i, [128, 512], F32) for i in range(2)]
            ysb = [sc.sb("ysb%d" % i, [128, 4, D], F32) for i in range(1)]
            yg = [sc.sb("yg%d" % i, [128, D], F32) for i in range(4)]
            xms = [sc.sb("xm%d" % i, [128, D], F32) for i in range(2)]
            accF, BaccF = sc.sb("accF", [128, D], F32)
            tf_, Btf = sc.sb("tFs", [128, D], F32)
            x2_, Bx2b = sc.sb("x2s", [128, D], F32)
            ssF = [sc.sb("ssF%d" % i, [128, 4], F32) for i in range(2)]
            junkF, BjunkF = sc.sb("junkF", [128, D], BF16)
            gfbc, Bgf = sc.sb("gfbc", [128, D], F32)
            S.dma("sp", gfbc[:], gfin_d.partition_broadcast(128), writes=[Bgf])
            PF = [sc.ps("PF%d" % i, [128, 512], F32) for i in range(6)]
            PTx = [sc.ps("PTx%d" % i, [128, D], BF16) for i in range(2)]
            pfi = 0
            cpi = 0
            wsrcs = [w.rearrange("l e k f -> (l e k) f") for w in (we1_d, we3_d, we2_d)]
            for jb in range(NB):
                w1_, Bw1 = w1b[jb % 2]
                w3_, Bw3 = w3b[jb % 2]
                w2_, Bw2 = w2b[0]
                for (wt, Bw, src) in ((w1_, Bw1, wsrcs[0]), (w3_, Bw3, wsrcs[1]), (w2_, Bw2, wsrcs[2])):
                    for kc in range(8):
                        S.idma(out=wt[:, kc, :], out_offset=None, in_=src,
                               in_offset=bass.IndirectOffsetOnAxis(ap=idxw[:, jb, kc:kc + 1], axis=0), reads=[Bidxw], writes=[Bw])
                for sub in range(2):
                  r0 = jb * BSL + sub * 512
                  xs_, Bxs_ = xst[sub]
                  S.dma("sp", xs_[:], XS[r0:r0 + 512, :].rearrange("(st p) d -> p st d", p=128), reads=[BXS], writes=[Bxs_])
                  for st in range(4):
                      ptx, Bptx = PTx[st % 2]
                      for kc in range(8):
                          S.op("pe", lambda kc=kc, st=st, ptx=ptx: nc.tensor.transpose(
                              out=ptx[:, kc * 128:(kc + 1) * 128], in_=xs_[:, st, kc * 128:(kc + 1) * 128], identity=identb[:]),
                              reads=[Bxs_, Bident], writes=[Bptx], sig=(kc == 7))
                      cpi += 1
                      if cpi % 2 == 0:
                          S.op("dve", lambda st=st, ptx=ptx: nc.vector.tensor_copy(
                              out=xT[:, :, st * 128:(st + 1) * 128], in_=ptx[:, :].rearrange("p (k c) -> p k c", c=128)), reads=[Bptx], writes=[BxT])
                      else:
                          S.op("act", lambda st=st, ptx=ptx: nc.scalar.copy(
                              out=xT[:, :, st * 128:(st + 1) * 128], in_=ptx[:, :].rearrange("p (k c) -> p k c", c=128)), reads=[Bptx], writes=[BxT])
                  for fc in range(8):
                      p1, Bp1 = PF[pfi % 6]
                      pfi += 1
                      p3, Bp3 = PF[pfi % 6]
                      pfi += 1
                      for kc in range(8):
                          S.op("pe", lambda kc=kc, fc=fc, p1=p1: nc.tensor.matmul(
                              p1[:, :], lhsT=w1_[:, kc, fc * 128:(fc + 1) * 128], rhs=xT[:, kc, :],
                              start=(kc == 0), stop=(kc == 7)), reads=[Bw1, BxT], writes=[Bp1], sig=(kc == 7))
                      for kc in range(8):
                          S.op("pe", lambda kc=kc, fc=fc, p3=p3: nc.tensor.matmul(
                              p3[:, :], lhsT=w3_[:, kc, fc * 128:(fc + 1) * 128], rhs=xT[:, kc, :],
                              start=(kc == 0), stop=(kc == 7)), reads=[Bw3, BxT], writes=[Bp3], sig=(kc == 7))
                      ts_, Bts = tS[fc % 2]
                      S.op("act", lambda p1=p1, ts_=ts_: nc.scalar.activation(out=ts_[:], in_=p1[:, :], func=AF.Silu), reads=[Bp1], writes=[Bts])
                      S.op("dve", lambda fc=fc, p3=p3, ts_=ts_: nc.vector.tensor_tensor(out=aT_[:, fc, :], in0=ts_[:], in1=p3[:, :], op=ALU.mult),
                           reads=[Bts, Bp3], writes=[BaT])
                  ys_, Bys_ = ysb[0]
                  for st in range(4):
                      for half in range(2):
                          py, Bpy = PF[pfi % 6]
                          pfi += 1
                          for fc in range(8):
                              S.op("pe", lambda fc=fc, st=st, half=half, py=py: nc.tensor.matmul(
                                  py[:, :], lhsT=aT_[:, fc, st * 128:(st + 1) * 128], rhs=w2_[:, fc, half * 512:(half + 1) * 512],
                                  start=(fc == 0), stop=(fc == 7)), reads=[BaT, Bw2], writes=[Bpy], sig=(fc == 7))
                          cpi += 1
                          if cpi % 2 == 0:
                              S.op("dve", lambda st=st, half=half, py=py: nc.vector.tensor_copy(out=ys_[:, st, half * 512:(half + 1) * 512], in_=py[:, :]),
                                   reads=[Bpy], writes=[Bys_])
                          else:
                              S.op("act", lambda st=st, half=half, py=py: nc.scalar.copy(out=ys_[:, st, half * 512:(half + 1) * 512], in_=py[:, :]),
                                   reads=[Bpy], writes=[Bys_])
                  S.dma("sp", YS[r0:r0 + 512, :].rearrange("(st p) d -> p st d", p=128), ys_[:], reads=[Bys_], writes=[BYS])
            S.barrier()
            for t in range(t_lo, NT):
                jj = 1 if t < 2 else 0
                y1, By1 = yg[2 * (t % 2)]
                y2, By2 = yg[2 * (t % 2) + 1]
                xm, Bxm = xms[t % 2]
                S.idma(out=y1[:], out_offset=None, in_=YS, in_offset=bass.IndirectOffsetOnAxis(ap=d1i[:, t:t + 1], axis=0), reads=[BYS, Bd1i], writes=[By1])
                S.idma(out=y2[:], out_offset=None, in_=YS, in_offset=bass.IndirectOffsetOnAxis(ap=d2i[:, t:t + 1], axis=0), reads=[BYS, Bd2i], writes=[By2])
                S.dma("sp", xm[:], XMID[t * 128:(t + 1) * 128, :], reads=[BXMID], writes=[Bxm])
                S.op("dve", lambda t=t: nc.vector.tensor_scalar(out=accF[:], in0=y1[:], scalar1=g12[:, 0, t:t + 1], scalar2=None, op0=ALU.mult),
                     reads=[By1, Bg12], writes=[BaccF])
                S.op("dve", lambda t=t: nc.vector.scalar_tensor_tensor(out=accF[:], in0=y2[:], scalar=g12[:, 1, t:t + 1], in1=accF[:],
                                                                        op0=ALU.mult, op1=ALU.add), reads=[By2, Bg12, BaccF], writes=[BaccF])
                S.op("dve", lambda jj=jj: nc.vector.tensor_tensor(out=tf_[:], in0=accF[:], in1=gt2bc[:, jj, :], op=ALU.mult),
                     reads=[BaccF, Bgt2], writes=[Btf])
                S.op("pool", lambda: nc.gpsimd.tensor_tensor(out=x2_[:], in0=tf_[:], in1=xm[:], op=ALU.add), reads=[Btf, Bxm], writes=[Bx2b])
                if not last:
                    S.dma("sp", XRES[t * 128:(t + 1) * 128, :], x2_[:], reads=[Bx2b], writes=[BXRES])
                else:
                    ss_, Bss_ = ssF[t % 2]
                    S.op("act", lambda ss_=ss_: nc.scalar.activation(out=junkF[:], in_=x2_[:], func=AF.Square, accum_out=ss_[:, 0:1]),
                         reads=[Bx2b], writes=[BjunkF, Bss_])
                    S.op("act", lambda ss_=ss_: nc.scalar.activation(out=ss_[:, 1:2], in_=ss_[:, 0:1], func=AF.Sqrt, scale=1.0 / D, bias=EPS),
                         reads=[Bss_], writes=[Bss_])
                    S.op("dve", lambda ss_=ss_: nc.vector.reciprocal(out=ss_[:, 2:3], in_=ss_[:, 1:2]), reads=[Bss_], writes=[Bss_])
                    S.op("dve", lambda ss_=ss_: nc.vector.scalar_tensor_tensor(
                        out=tf_[:], in0=x2_[:], scalar=ss_[:, 2:3], in1=gfbc[:], op0=ALU.mult, op1=ALU.mult),
                        reads=[Bx2b, Bss_, Bgf], writes=[Btf])
                    S.dma("sp", out_d[(t - 2) * 128:(t - 1) * 128, :], tf_[:], reads=[Btf], writes=[Bout])
            S.barrier()
            sc.close()
            if stop_here("F%d" % li):
                done = True
                break
            continue
        NPASS = 3
        per = (ntl + NPASS - 1) // NPASS
        w1b = [sc.sb("w1b%d" % i, [128, 8, D], BF16) for i in range(2)]
        w3b = [sc.sb("w3b%d" % i, [128, 8, D], BF16) for i in range(2)]
        w2b = [sc.sb("w2b%d" % i, [128, 8, D], BF16) for i in range(1)]
        h2p, Bh2p = sc.sb("h2p", [128, 8, per * 128], BF16)
        yacc, Byacc = sc.sb("yacc", [128, per, D], F32)
        aT = [sc.sb("aT%d" % i, [128, 8, 512], BF16) for i in range(1)]
        tS = [sc.sb("tS%d" % i, [128, 512], F32) for i in range(2)]
        xm = [sc.sb("xm%d" % i, [128, D], F32) for i in range(1)]
        tF = [sc.sb("tF%d" % i, [128, D], F32) for i in range(1)]
        x2b = [sc.sb("x2b%d" % i, [128, D], F32) for i in range(1)]
        ssF = [sc.sb("ssF%d" % i, [128, 4], F32) for i in range(2)]
        junkF, BjunkF = sc.sb("junkF", [128, D], BF16)
        gfbc, Bgf = sc.sb("gfbc", [128, D], F32)
        S.dma("sp", gfbc[:], gfin_d.partition_broadcast(128), writes=[Bgf])
        PF = [sc.ps("PF%d" % i, [128, 512], F32) for i in range(8)]
        pfi = 0
        wli = 0
        for ps_ in range(NPASS):
            ta_ = t_lo + ps_ * per
            tb2 = min(NT, ta_ + per)
            npt = tb2 - ta_
            if npt <= 0:
                continue
            S.dma("sp", h2p[:, :, 0:npt * 128], H2T[:, :, ta_ * 128:tb2 * 128], reads=[BH2T], writes=[Bh2p])
            pchunks = []
            t = 0
            while t < npt:
                nn = min(4, npt - t)
                pchunks.append((t, nn))
                t += nn
            for e in range(NE):
                w1_, Bw1 = w1b[wli % 2]
                w3_, Bw3 = w3b[wli % 2]
                w2_, Bw2 = w2b[0]
                wli += 1
                for (wt, Bw, src) in ((w1_, Bw1, we1_d), (w3_, Bw3, we3_d), (w2_, Bw2, we2_d)):
                    for hh in range(2):
                        S.dma("pool", wt[:, hh * 4:(hh + 1) * 4, :],
                              src[li, e].rearrange("(kc p) n -> p kc n", p=128)[:, hh * 4:(hh + 1) * 4, :], writes=[Bw])
                for cidx, (t0, nn) in enumerate(pchunks):
                    n = nn * 128
                    c0 = t0 * 128
                    at_, Bat = aT[0]
                    for fc in range(8):
                        p1, Bp1 = PF[pfi % 8]
                        pfi += 1
                        p3, Bp3 = PF[pfi % 8]
                        pfi += 1
                        for kc in range(8):
                            S.op("pe", lambda kc=kc, fc=fc, p1=p1: nc.tensor.matmul(
                                p1[:, 0:n], lhsT=w1_[:, kc, fc * 128:(fc + 1) * 128], rhs=h2p[:, kc, c0:c0 + n],
                                start=(kc == 0), stop=(kc == 7)), reads=[Bw1, Bh2p], writes=[Bp1], sig=(kc == 7))
                        for kc in range(8):
                            S.op("pe", lambda kc=kc, fc=fc, p3=p3: nc.tensor.matmul(
                                p3[:, 0:n], lhsT=w3_[:, kc, fc * 128:(fc + 1) * 128], rhs=h2p[:, kc, c0:c0 + n],
                                start=(kc == 0), stop=(kc == 7)), reads=[Bw3, Bh2p], writes=[Bp3], sig=(kc == 7))
                        ts_, Bts = tS[fc % 2]
                        S.op("act", lambda p1=p1, ts_=ts_: nc.scalar.activation(out=ts_[:, 0:n], in_=p1[:, 0:n], func=AF.Silu),
                             reads=[Bp1], writes=[Bts])
                        S.op("dve", lambda fc=fc, p3=p3, ts_=ts_: nc.vector.tensor_tensor(out=at_[:, fc, 0:n], in0=ts_[:, 0:n], in1=p3[:, 0:n], op=ALU.mult),
                             reads=[Bts, Bp3], writes=[Bat])
                    for j in range(nn):
                        lt = t0 + j
                        gt_ = ta_ + lt
                        for half in range(2):
                            py, Bpy = PF[pfi % 8]
                            pfi += 1
                            for fc in range(8):
                                S.op("pe", lambda fc=fc, j=j, half=half, py=py: nc.tensor.matmul(
                                    py[:, :], lhsT=at_[:, fc, j * 128:(j + 1) * 128], rhs=w2_[:, fc, half * 512:(half + 1) * 512],
                                    start=(fc == 0), stop=(fc == 7)), reads=[Bat, Bw2], writes=[Bpy], sig=(fc == 7))
                            if e == 0:
                                S.op("dve", lambda py=py, lt=lt, gt_=gt_, half=half: nc.vector.tensor_scalar(
                                    out=yacc[:, lt, half * 512:(half + 1) * 512], in0=py[:, :], scalar1=gates[:, gt_, e:e + 1], scalar2=None,
                                    op0=ALU.mult), reads=[Bpy, Bgates], writes=[Byacc])
                            else:
                                S.op("dve", lambda py=py, lt=lt, gt_=gt_, half=half, e=e: nc.vector.scalar_tensor_tensor(
                                    out=yacc[:, lt, half * 512:(half + 1) * 512], in0=py[:, :], scalar=gates[:, gt_, e:e + 1],
                                    in1=yacc[:, lt, half * 512:(half + 1) * 512], op0=ALU.mult, op1=ALU.add),
                                    reads=[Bpy, Bgates, Byacc], writes=[Byacc])
            for lt in range(npt):
                gt_ = ta_ + lt
                jj = 1 if gt_ < 2 else 0
                xm_, Bxm = xm[0]
                tf_, Btf = tF[0]
                x2_, Bx2b = x2b[0]
                S.dma("sp", xm_[:], XMID[gt_ * 128:(gt_ + 1) * 128, :], reads=[BXMID], writes=[Bxm])
                S.op("dve", lambda lt=lt, tf_=tf_, jj=jj: nc.vector.tensor_tensor(out=tf_[:], in0=yacc[:, lt, :], in1=gt2bc[:, jj, :], op=ALU.mult),
                     reads=[Byacc, Bgt2], writes=[Btf])
                S.op("pool", lambda tf_=tf_, xm_=xm_, x2_=x2_: nc.gpsimd.tensor_tensor(out=x2_[:], in0=tf_[:], in1=xm_[:], op=ALU.add),
                     reads=[Btf, Bxm], writes=[Bx2b])
                if not last:
                    S.dma("sp", XRES[gt_ * 128:(gt_ + 1) * 128, :], x2_[:], reads=[Bx2b], writes=[BXRES])
                else:
                    ss_, Bss_ = ssF[lt % 2]
                    S.op("act", lambda x2_=x2_, ss_=ss_: nc.scalar.activation(out=junkF[:], in_=x2_[:], func=AF.Square, accum_out=ss_[:, 0:1]),
                         reads=[Bx2b], writes=[BjunkF, Bss_])
                    S.op("act", lambda ss_=ss_: nc.scalar.activation(out=ss_[:, 1:2], in_=ss_[:, 0:1], func=AF.Sqrt, scale=1.0 / D, bias=EPS),
                         reads=[Bss_], writes=[Bss_])
                    S.op("dve", lambda ss_=ss_: nc.vector.reciprocal(out=ss_[:, 2:3], in_=ss_[:, 1:2]), reads=[Bss_], writes=[Bss_])
                    S.op("dve", lambda x2_=x2_, ss_=ss_, tf_=tf_: nc.vector.scalar_tensor_tensor(
                        out=tf_[:], in0=x2_[:], scalar=ss_[:, 2:3], in1=gfbc[:], op0=ALU.mult, op1=ALU.mult),
                        reads=[Bx2b, Bss_, Bgf], writes=[Btf])
                    S.dma("sp", out_d[(gt_ - 2) * 128:(gt_ - 1) * 128, :], tf_[:], reads=[Btf], writes=[Bout])
        S.barrier()
        sc.close()
        if stop_here("F%d" % li):
            done = True
            break

    S.barrier()
    build.stats = dict(S.ninstr, sems=len(S.dall), ccnt=dict(S.ccnt))
    build.marks = S.marks
    return nc


def _rope_tables():
    n_pairs = 16
    inv = (np.float32(10000.0) ** (-np.arange(n_pairs, dtype=np.float32) / np.float32(n_pairs))).astype(np.float32)
    t = np.arange(SEQ)
    r = (t // 64).astype(np.float32)
    col = (t % 64).astype(np.float32)
    ang = np.concatenate([r[:, None] * inv[None, :], col[:, None] * inv[None, :]], axis=-1).astype(np.float32)
    cos = np.cos(ang).astype(np.float32)
    sin = np.sin(ang).astype(np.float32)
    p = np.arange(128)
    pair = (p % 64) // 2
    sign = np.where(p % 2 == 0, -1.0, 1.0).astype(np.float32)
    ctab = np.ascontiguousarray(cos[:, pair].T)
    stab = np.ascontiguousarray((sin[:, pair] * sign[None, :]).T)
    return ctab, stab


def prepare_shared(inp):
    f = lambda a: np.ascontiguousarray(np.asarray(a, dtype=np.float32))
    w_in = f(inp["w_in"])
    kcols = np.arange(1024, 2048)
    qcols = np.arange(3072, 4096)
    swap = lambda c: c ^ 1
    w_in_ext = np.concatenate([w_in, w_in[:, :, swap(kcols)], w_in[:, :, swap(qcols)]], axis=2)
    b_mod = f(inp["b_mod"])
    bm = b_mod.reshape(DEPTH, 6, 8, 128)[:, [0, 1, 3, 4]]
    bmodP = np.repeat(bm.transpose(0, 3, 1, 2)[..., None], 2, axis=-1)
    gn = np.stack([f(inp["g_norm1"]).reshape(DEPTH, 8, 128), f(inp["g_norm2"]).reshape(DEPTH, 8, 128)], axis=1)
    gn = np.repeat(gn.transpose(0, 3, 1, 2)[..., None], 2, axis=-1)
    ctab, stab = _rope_tables()
    cw = f(inp["conv_w"]).reshape(DEPTH, 4, 8, 128).transpose(0, 3, 2, 1)
    cb = f(inp["conv_b"]).reshape(DEPTH, 8, 128).transpose(0, 2, 1)
    lwa = f(inp["lru_wa"]).transpose(0, 3, 1, 2, 4).reshape(DEPTH, 128, 16 * 128)
    lwx = f(inp["lru_wx"]).transpose(0, 3, 1, 2, 4).reshape(DEPTH, 128, 16 * 128)
    v16 = lambda a: f(a).reshape(DEPTH, 2, 8, 128).transpose(0, 3, 1, 2).reshape(DEPTH, 128, 16)
    shared = {
        "w_mod": f(inp["w_mod"]), "b_mod": b_mod, "bmodP": f(bmodP), "gn": f(gn),
        "w_in_ext": f(w_in_ext), "ctab": ctab, "stab": stab, "ident": np.eye(128, dtype=np.float32),
        "cw": f(cw), "cb": f(cb), "lwa": f(lwa), "lwx": f(lwx),
        "lba": v16(inp["lru_ba"]), "lbx": v16(inp["lru_bx"]), "llam": v16(inp["lru_lambda"]),
        "dlam": f(inp["diff_lambda"]).reshape(DEPTH, 256), "gsub": f(inp["g_subln"]),
        "w_rnn_proj": f(inp["w_rnn_proj"]), "w_attn_proj": f(inp["w_attn_proj"]), "w_o": f(inp["w_o"]),
        "w_router": f(inp["w_router"]), "b_router": f(inp["b_router"]),
        "w_e1": f(inp["w_e1"]), "w_e3": f(inp["w_e3"]), "w_e2": f(inp["w_e2"]),
        "g_final": f(inp["g_final"]),
        "utri": np.triu(np.ones((128, 128), np.float32), 1), "onesm": np.ones((128, 128), np.float32),
        "prow": (np.arange(8)[None, :] * 128 + np.arange(128)[:, None]).astype(np.float32),
        "thr": np.tile((512.0 * np.arange(64, dtype=np.float32))[None, :], (128, 1)),
    }
    return shared


def prepare_core(inp, b):
    x = np.asarray(inp["x"], dtype=np.float32)
    ctx = np.asarray(inp["ctx"], dtype=np.float32)
    c = np.asarray(inp["c"], dtype=np.float32)
    c_ctx = np.asarray(inp["c_ctx"], dtype=np.float32)
    x_in = np.ascontiguousarray(np.concatenate([ctx[b], x[b]], axis=0))
    cc = np.stack([c[b].reshape(8, 128).T, c_ctx.reshape(8, 128).T], axis=-1)
    return {"x_in": x_in, "cc": np.ascontiguousarray(cc.astype(np.float32))}


def kernel(**inputs):
    nb = np.asarray(inputs["x"]).shape[0]
    shared = prepare_shared(inputs)
    nc = build()
    in_maps = []
    for b in range(nb):
        m = dict(shared)
        m.update(prepare_core(inputs, b))
        in_maps.append(m)
    res = run_bass_kernel_spmd(nc, in_maps, core_ids=list(range(nb)))
    return np.stack([np.asarray(r["out"], dtype=np.float32) for r in res.results], axis=0)
```

```python
import os
import math
import numpy as np
import concourse.bass as bass
import concourse.mybir as mybir
from concourse.bass_utils import run_bass_kernel_spmd

F32 = mybir.dt.float32
BF16 = mybir.dt.bfloat16
AF = mybir.ActivationFunctionType
ALU = mybir.AluOpType
AX = mybir.AxisListType

D = 1024
LC = 256
SEQ = 4096
TOK = LC + SEQ
NT = TOK // 128
DEPTH = 2
NE = 16
EPS = 1e-6
NEXT = 9216
DBG = {}
MOE_SPARSE = True
I32 = mybir.dt.int32
NBMAX = 25
BSL = 1024


class Buf:
    __slots__ = ("name", "writer", "readers", "lsem", "ssem", "pend")

    def __init__(self, name):
        self.name = name
        self.writer = None
        self.readers = []
        self.lsem = None
        self.ssem = None
        self.pend = False


class Sched:
    COMPUTE = ("pe", "act", "dve", "pool")

    def __init__(self, nc):
        self.nc = nc
        self.eng = {"pe": nc.tensor, "act": nc.scalar, "dve": nc.vector,
                    "pool": nc.gpsimd, "sp": nc.sync}
        self.csem = {}
        self.ccnt = {}
        self._stack = []
        for e in self.COMPUTE:
            g = nc.semaphore("cs_" + e)
            self.csem[e] = g.__enter__()
            self._stack.append(g)
            self.ccnt[e] = 0
        self.seen = {e: {} for e in self.eng}
        self.pe_pending = []
        self.pe_pending_w = []
        self.dpool = []
        self.dall = []
        self.dholders = []
        self.ninstr = {e: 0 for e in self.eng}

    def _getdsem(self, q):
        kind = "sw" if q == "pool" else "hw"
        for i, d in enumerate(self.dpool):
            if d["kind"] == kind:
                return self.dpool.pop(i)
        i = len(self.dall)
        g = self.nc.semaphore("ds_%d" % i)
        s = g.__enter__()
        self._stack.append(g)
        d = {"sem": s, "cnt": 0, "key": i, "kind": kind}
        self.dall.append(d)
        return d

    def _wait(self, consumer, key, sem, val):
        d = self.seen[consumer]
        if d.get(key, 0) >= val:
            return
        d[key] = val
        self.eng[consumer].wait_ge(sem, val)
        self.ninstr[consumer] += 1

    def _wait_rec(self, consumer, rec):
        if rec is None:
            return
        if rec[0] == "c":
            _, e, seq = rec
            if e == "pe" and consumer == "pe":
                return
            assert seq is not None, "dependency on unsignaled instr"
            self._wait(consumer, ("c", e), self.csem[e], seq)
        else:
            _, sem, cnt, key = rec
            self._wait(consumer, ("d", key), sem, cnt)

    def _deps(self, consumer, reads, writes):
        for b in reads:
            if b.writer is not None and b.writer[0] == "c" and b.writer[2] is None:
                assert consumer == "pe", "read of %s whose PE writer is unsignaled" % b.name
            self._wait_rec(consumer, b.writer)
        for b in writes:
            assert not (b.pend and consumer != "pe"), "write to %s with pending unsignaled PE read" % b.name
            if b.writer is not None and b.writer[0] == "c" and b.writer[2] is None:
                assert consumer == "pe", "write to %s whose PE writer is unsignaled" % b.name
            self._wait_rec(consumer, b.writer)
            for r in b.readers:
                self._wait_rec(consumer, r)

    @staticmethod
    def _trim(b):
        if len(b.readers) > 8:
            last = {}
            for r in b.readers:
                k = (r[0], r[1]) if r[0] == "c" else (r[0], r[3])
                last[k] = r
            b.readers = list(last.values())

    def op(self, e, fn, reads=(), writes=(), sig=True):
        self._deps(e, reads, writes)
        ins = fn()
        self.ninstr[e] += 1
        if sig:
            self.ccnt[e] += 1
            seq = self.ccnt[e]
            ins.then_inc(self.csem[e], 1)
            rec = ("c", e, seq)
            if e == "pe":
                for b in self.pe_pending:
                    b.readers.append(rec)
                    b.pend = False
                self.pe_pending = []
                for b in self.pe_pending_w:
                    b.writer = rec
                self.pe_pending_w = []
        else:
            assert e == "pe"
            rec = ("c", e, None)
        for b in writes:
            b.writer = rec
            b.readers = []
            if rec[2] is None:
                self.pe_pending_w.append(b)
        for b in reads:
            if b in writes:
                continue
            if rec[2] is None:
                if not b.pend:
                    b.pend = True
                    self.pe_pending.append(b)
            else:
                b.readers.append(rec)
                self._trim(b)
        return ins

    def dma(self, q, out, in_, reads=(), writes=(), **kw):
        self._deps(q, reads, writes)
        ins = self.eng[q].dma_start(out=out, in_=in_, **kw)
        self.ninstr[q] += 1
        if writes:
            b = writes[0]
            if b.lsem is None or b.lsem["kind"] != ("sw" if q == "pool" else "hw"):
                b.lsem = self._getdsem(q)
                self.dholders.append(b)
            d = b.lsem
        else:
            b = reads[0]
            if b.ssem is None or b.ssem["kind"] != ("sw" if q == "pool" else "hw"):
                b.ssem = self._getdsem(q)
                self.dholders.append(b)
            d = b.ssem
        d["cnt"] += 16
        ins.then_inc(d["sem"], 16)
        rec = ("d", d["sem"], d["cnt"], d["key"])
        for b in writes:
            b.writer = rec
            b.readers = []
        for b in reads:
            b.readers.append(rec)
            self._trim(b)
        return ins

    def idma(self, out, out_offset, in_, in_offset, reads=(), writes=()):
        q = "pool"
        self._deps(q, reads, writes)
        ins = self.nc.gpsimd.indirect_dma_start(out=out, out_offset=out_offset, in_=in_, in_offset=in_offset)
        self.ninstr[q] += 1
        b = writes[0]
        if b.lsem is None or b.lsem["kind"] != "sw":
            b.lsem = self._getdsem(q)
            self.dholders.append(b)
        d = b.lsem
        d["cnt"] += 16
        ins.then_inc(d["sem"], 16)
        rec = ("d", d["sem"], d["cnt"], d["key"])
        for b in writes:
            b.writer = rec
            b.readers = []
        for b in reads:
            b.readers.append(rec)
            self._trim(b)
        return ins

    def wait_buf(self, consumer, b):
        self._wait_rec(consumer, b.writer)
        for r in b.readers:
            self._wait_rec(consumer, r)

    def barrier(self, engines=None):
        assert not self.pe_pending and not self.pe_pending_w, "unsignaled PE work at barrier"
        self.marks = getattr(self, "marks", []) + [dict(self.ccnt)]
        for q in (engines or self.eng):
            for e in self.COMPUTE:
                if self.ccnt[e] > 0:
                    self._wait(q, ("c", e), self.csem[e], self.ccnt[e])
            for d in self.dall:
                if d["cnt"] > 0:
                    self._wait(q, ("d", d["key"]), d["sem"], d["cnt"])
        for b in self.dholders:
            b.lsem = None
            b.ssem = None
        self.dholders = []
        self.dpool = list(self.dall)

    def close(self):
        for g in reversed(self._stack):
            g.__exit__(None, None, None)


class Scope:
    def __init__(self, nc, tag):
        self.nc = nc
        self.tag = tag
        self.gs = []
        self.n = 0

    def sb(self, name, shape, dt):
        g = self.nc.sbuf_tensor("%s_%s" % (name, self.tag), list(shape), dt)
        t = g.__enter__()
        self.gs.append(g)
        return t, Buf(name)

    def ps(self, name, shape, dt):
        g = self.nc.psum_tensor("%s_%s" % (name, self.tag), list(shape), dt)
        t = g.__enter__()
        self.gs.append(g)
        return t, Buf(name)

    def close(self):
        for g in reversed(self.gs):
            g.__exit__(None, None, None)
        self.gs = []


def token_chunks(last):
    ch = []
    if not last:
        ch.append((0, LC, True))
    for i in range(SEQ // 512):
        ch.append((LC + i * 512, 512, False))
    return ch


def build(debug_stop=None, n_layers=DEPTH):
    nc = bass.Bass("TRN2", target_bir_lowering=False)
    S = Sched(nc)
    dbg = debug_stop is not None
    skind = "ExternalOutput" if dbg else "Internal"

    def din(name, shape, dt=F32):
        return nc.dram_tensor(name, list(shape), dt, kind="ExternalInput").ap()

    def dscr(name, shape, dt):
        return nc.dram_tensor(name, list(shape), dt, kind=skind).ap(), Buf(name)

    x_in = din("x_in", [TOK, D])
    cc_d = din("cc", [128, 8, 2])
    w_mod = din("w_mod", [DEPTH, D, 6 * D])
    b_mod = din("b_mod", [DEPTH, 6 * D])
    bmodP_d = din("bmodP", [DEPTH, 128, 4, 8, 2])
    gn_d = din("gn", [DEPTH, 128, 2, 8, 2])
    w_in = din("w_in_ext", [DEPTH, D, NEXT])
    ctab_d = din("ctab", [128, SEQ])
    stab_d = din("stab", [128, SEQ])
    ident_d = din("ident", [128, 128])
    cw_d = din("cw", [DEPTH, 128, 8, 4])
    cb_d = din("cb", [DEPTH, 128, 8])
    lwa_d = din("lwa", [DEPTH, 128, 16 * 128])
    lwx_d = din("lwx", [DEPTH, 128, 16 * 128])
    lba_d = din("lba", [DEPTH, 128, 16])
    lbx_d = din("lbx", [DEPTH, 128, 16])
    llam_d = din("llam", [DEPTH, 128, 16])
    dlam_d = din("dlam", [DEPTH, 256])
    gsub_d = din("gsub", [DEPTH, 128])
    wrp_d = din("w_rnn_proj", [DEPTH, D, D])
    wap_d = din("w_attn_proj", [DEPTH, D, D])
    wo_d = din("w_o", [DEPTH, D, D])
    wrt_d = din("w_router", [D, NE])
    brt_d = din("b_router", [NE])
    we1_d = din("w_e1", [DEPTH, NE, D, D])
    we3_d = din("w_e3", [DEPTH, NE, D, D])
    we2_d = din("w_e2", [DEPTH, NE, D, D])
    gfin_d = din("g_final", [D])
    utri_d = din("utri", [128, 128])
    onesm_d = din("onesm", [128, 128])
    prow_d = din("prow", [128, 8])
    thr_d = din("thr", [128, 64])
    out_d = nc.dram_tensor("out", [SEQ, D], F32, kind="ExternalOutput").ap()
    Bout = Buf("out")

    XMID, BXMID = dscr("XMID", [TOK, D], F32)
    XRES, BXRES = dscr("XRES", [TOK, D], F32)
    RX, BRX = dscr("RX", [8, 128, TOK], F32)
    KT, BKT = dscr("KT", [8, 128, TOK], BF16)
    QT, BQT = dscr("QT", [8, 128, TOK], BF16)
    VV, BVV = dscr("VV", [TOK, D], BF16)
    GRG, BGRG = dscr("GRG", [8, 128, TOK], BF16)
    SGR, BSGR = dscr("SGR", [8, 128, TOK], BF16)
    SGA, BSGA = dscr("SGA", [8, 128, TOK], BF16)
    OAT, BOAT = dscr("OAT", [8, 128, TOK], BF16)
    ZT, BZT = dscr("ZT", [8, 128, TOK], BF16)
    H2T, BH2T = dscr("H2T", [128, 8, TOK], BF16)
    H2TOK, BH2TOK = dscr("H2TOK", [TOK, D], BF16)
    XS, BXS = dscr("XS", [NBMAX * BSL, D], BF16)
    YS, BYS = dscr("YS", [NBMAX * BSL, D], F32)

    P = Scope(nc, "P")
    identb, Bident = P.sb("identb", [128, 128], BF16)
    sil, Bsil = P.sb("sil", [128, 8, 2], F32)
    gt1bc, Bgt1 = P.sb("gt1bc", [128, 2, D], F32)
    gt2bc, Bgt2 = P.sb("gt2bc", [128, 2, D], F32)
    modP, BmodP = P.sb("modP", [128, 4, 8, 2], F32)
    AA, BAA = P.sb("AA", [128, 2, 8, 2], F32)
    logits, Blog = P.sb("logits", [128, NT, NE], F32)
    gates, Bgates = P.sb("gates", [128, NT, NE], F32)
    brtbc, Bbrt = P.sb("brtbc", [128, NE], F32)
    wrt, Bwrt = P.sb("wrt", [128, 8, NE], BF16)

    utri16, Butri = P.sb("utri16", [128, 128], BF16)
    ones16m, Bonesm = P.sb("ones16m", [128, 128], BF16)
    prow, Bprow = P.sb("prow", [128, 8], F32)
    thr, Bthr = P.sb("thr", [128, 64], F32)
    S.dma("pool", utri16[:], utri_d, writes=[Butri])
    S.dma("pool", ones16m[:], onesm_d, writes=[Bonesm])
    S.dma("sp", prow[:], prow_d, writes=[Bprow])
    S.dma("sp", thr[:], thr_d, writes=[Bthr])
    S.dma("pool", identb[:], ident_d, writes=[Bident])
    S.dma("sp", sil[:], cc_d, writes=[Bsil])
    S.dma("sp", brtbc[:], brt_d.partition_broadcast(128), writes=[Bbrt])
    S.dma("pool", wrt[:], wrt_d.rearrange("(kc p) e -> p kc e", p=128), writes=[Bwrt])
    S.op("act", lambda: nc.scalar.activation(out=sil[:], in_=sil[:], func=AF.Silu), reads=[Bsil], writes=[Bsil])

    def rmsnorm_T(sc, x_ap, Bx, li, which, jj, dst_fn, Bdst, PT, BPT, tmp):
        junk, Bjunk, ss, Bss, xn, Bxn = tmp
        S.op("act", lambda: nc.scalar.activation(out=junk[:], in_=x_ap, func=AF.Square, accum_out=ss[:, 0:1]),
             reads=[Bx], writes=[Bjunk, Bss])
        S.op("act", lambda: nc.scalar.activation(out=ss[:, 1:2], in_=ss[:, 0:1], func=AF.Sqrt, scale=1.0 / D, bias=EPS),
             reads=[Bss], writes=[Bss])
        S.op("dve", lambda: nc.vector.reciprocal(out=ss[:, 2:3], in_=ss[:, 1:2]), reads=[Bss], writes=[Bss])
        S.op("act", lambda: nc.scalar.activation(out=xn[:], in_=x_ap, func=AF.Identity, scale=ss[:, 2:3]),
             reads=[Bx, Bss], writes=[Bxn])
        for kc in range(8):
            S.op("pe", lambda kc=kc: nc.tensor.transpose(out=PT[:, kc * 128:(kc + 1) * 128],
                                                          in_=xn[:, kc * 128:(kc + 1) * 128], identity=identb[:]),
                 reads=[Bxn, Bident], writes=[BPT], sig=(kc == 7))
        for kc in range(8):
            S.op("dve", lambda kc=kc: nc.vector.tensor_scalar(
                out=dst_fn(kc), in0=PT[:, kc * 128:(kc + 1) * 128],
                scalar1=AA[:, which, kc, jj:jj + 1], scalar2=modP[:, 2 * which, kc, jj:jj + 1],
                op0=ALU.mult, op1=ALU.add), reads=[BPT, BAA, BmodP], writes=[Bdst])

    def stop_here(name):
        return dbg and debug_stop == name

    done = False
    for li in range(n_layers):
        if li in DBG.get("skip_layers", ()):
            continue
        last = li == DEPTH - 1
        lam_init = 0.8 - 0.6 * math.exp(-0.3 * li)
        xcur, Bxcur = (x_in, Buf("x_in")) if li == 0 else (XRES, BXRES)
        chunks = token_chunks(last)

        skipAB = DBG.get("skipAB", False)
        sc = Scope(nc, "A%d" % li)
        wm = [sc.sb("wm%d" % i, [128, 8, 512], F32) for i in range(2)]
        silbc, Bsilbc = sc.sb("silbc", [128, 8, 2, 128], F32)
        for kc in range(8):
            for j in range(2):
                S.op("dve", lambda kc=kc, j=j: nc.vector.tensor_copy(
                    out=silbc[:, kc, j, :], in_=sil[:, kc, j:j + 1].to_broadcast([128, 128])),
                    reads=[Bsil], writes=[Bsilbc])
        bmg, Bbmg = sc.sb("bmg", [128, 2048], F32)
        bmp, Bbmp = sc.sb("bmp", [128, 4, 8, 2], F32)
        gnt, Bgnt = sc.sb("gnt", [128, 2, 8, 2], F32)
        pmod, Bpmod = sc.ps("pmod", [128, 512], F32)
        pg = [sc.ps("pg%d" % i, [128, 512], F32) for i in range(2)]
        S.dma("sp", bmg[:, 0:1024], b_mod[li, 2048:3072].partition_broadcast(128), writes=[Bbmg])
        S.dma("sp", bmg[:, 1024:2048], b_mod[li, 5120:6144].partition_broadcast(128), writes=[Bbmg])
        S.dma("sp", bmp[:], bmodP_d[li], writes=[Bbmp])
        S.dma("sp", gnt[:], gn_d[li], writes=[Bgnt])
        wm_src = w_mod[li].rearrange("(kc p) n -> p kc n", p=128)
        ppi = 0
        gi = 0
        for blk in range(12):
            wt, Bw = wm[blk % 2]
            S.dma("sp", wt[:], wm_src[:, :, blk * 512:(blk + 1) * 512], writes=[Bw])
            if blk in (4, 5, 10, 11):
                gdst, Bgd = (gt1bc, Bgt1) if blk < 6 else (gt2bc, Bgt2)
                half = blk % 2
                bo = (0 if blk < 6 else 1024) + half * 512
                for j in range(2):
                    pt_, Bp = pg[gi % 2]
                    gi += 1
                    for kc in range(8):
                        S.op("pe", lambda kc=kc, j=j, pt_=pt_, wt=wt: nc.tensor.matmul(
                            pt_[:, :], lhsT=silbc[:, kc, j, :], rhs=wt[:, kc, :], start=(kc == 0), stop=(kc == 7)),
                            reads=[Bsilbc, Bw], writes=[Bp], sig=(kc == 7))
                    S.op("dve", lambda j=j, pt_=pt_, gdst=gdst, half=half, bo=bo: nc.vector.tensor_tensor(
                        out=gdst[:, j, half * 512:(half + 1) * 512], in0=pt_[:, :], in1=bmg[:, bo:bo + 512], op=ALU.add),
                        reads=[Bp, Bbmg], writes=[Bgd])
            else:
                for cch in range(4):
                    for kc in range(8):
                        S.op("pe", lambda kc=kc, cch=cch, wt=wt, ppi=ppi: nc.tensor.matmul(
                            pmod[:, 2 * ppi:2 * ppi + 2], lhsT=wt[:, kc, cch * 128:(cch + 1) * 128], rhs=sil[:, kc, :],
                            start=(kc == 0), stop=(kc == 7)),
                            reads=[Bw, Bsil], writes=[Bpmod], sig=(kc == 7))
                    ppi += 1
        assert ppi == 32
        if True:
          S.op("dve", lambda: nc.vector.tensor_tensor(
            out=modP[:].rearrange("p a b c -> p (a b c)"), in0=pmod[:, 0:64],
            in1=bmp[:].rearrange("p a b c -> p (a b c)"), op=ALU.add), reads=[Bpmod, Bbmp], writes=[BmodP])
        for which in range(2):
            S.op("dve", lambda which=which: nc.vector.scalar_tensor_tensor(
                out=AA[:, which].rearrange("p b c -> p (b c)"),
                in0=modP[:, 2 * which + 1].rearrange("p b c -> p (b c)"), scalar=1.0,
                in1=gnt[:, which].rearrange("p b c -> p (b c)"), op0=ALU.add, op1=ALU.mult),
                reads=[BmodP, Bgnt], writes=[BAA])
        S.barrier()
        sc.close()
        if stop_here("A%d" % li):
            done = True
            break

        scB = Scope(nc, "B%d" % li)
        hT, BhT = scB.sb("hT", [128, 8, TOK], BF16)
        sc = Scope(nc, "B1%d" % li)
        xt = [sc.sb("xt%d" % i, [128, D], F32) for i in range(3)]
        tmps = [(sc.sb("junk%d" % i, [128, D], BF16) + sc.sb("ss%d" % i, [128, 4], F32) + sc.sb("xn%d" % i, [128, D], BF16))
                for i in range(2)]
        PTs = [sc.ps("PT%d" % i, [128, D], BF16) for i in range(2)]
        for t in range(0 if skipAB else NT):
            if last and False:
                pass
            xtt, Bxt = xt[t % 3]
            S.dma("sp", xtt[:], xcur[t * 128:(t + 1) * 128, :], reads=[Bxcur], writes=[Bxt])
            PT, BPT = PTs[t % 2]
            jj = 1 if t < 2 else 0
            rmsnorm_T(sc, xtt[:], Bxt, li, 0, jj,
                      lambda kc, t=t: hT[:, kc, t * 128:(t + 1) * 128], BhT, PT, BPT, tmps[t % 2])
        S.barrier()
        sc.close()

        sc = Scope(nc, "B2%d" % li)
        wb = [sc.sb("wb%d" % i, [128, 8, 512], BF16) for i in range(4)]
        ctab, Bctab = sc.sb("ctab", [128, SEQ], BF16)
        stab, Bstab = sc.sb("stab", [128, SEQ], BF16)
        st32, Bst32 = sc.sb("st32", [128, TOK], F32)
        st16 = [sc.sb("st16_%d" % i, [128, TOK], BF16) for i in range(2)]
        t1s = [sc.sb("t1_%d" % i, [128, 512], F32) for i in range(2)]
        t2s = [sc.sb("t2_%d" % i, [128, 512], F32) for i in range(2)]
        stv = [sc.sb("stv%d" % i, [128, 512], BF16) for i in range(4)]
        PB = [sc.ps("PB%d" % i, [128, 512], F32) for i in range(8)]
        S.dma("pool", ctab[:], ctab_d, writes=[Bctab])
        S.dma("pool", stab[:], stab_d, writes=[Bstab])
        win_src = w_in[li].rearrange("(kc p) n -> p kc n", p=128)
        jobs = [(0,), (1,), (2, 14), (3, 15), (4,), (5,), (6, 16), (7, 17), (8,), (9,), (10,), (11,), (12,), (13,)]
        loads = [b for jb in jobs for b in jb]
        slot_of = {}
        nload = [0]

        def load_next():
            if nload[0] < len(loads):
                b = loads[nload[0]]
                wt, Bw = wb[nload[0] % 4]
                slot_of[b] = nload[0] % 4
                S.dma("pool", wt[:], win_src[:, :, b * 512:(b + 1) * 512], writes=[Bw])
                nload[0] += 1

        cum = []
        acc_ = 0
        for jb in jobs:
            acc_ += len(jb)
            cum.append(acc_)
        pbi = [0]
        s16i = [0]
        tti = [0]
        evi = [0]

        def nextpb():
            r = PB[pbi[0] % 8]
            pbi[0] += 1
            return r

        for ji, jb in enumerate([] if skipAB else jobs):
            while nload[0] < cum[min(ji + 1, len(jobs) - 1)]:
                load_next()
            b0 = jb[0]
            part = b0 // 2
            w0, Bw0 = wb[slot_of[b0]]
            if part == 2:
                tiles = range(NT)
                for tt in tiles:
                    pt_, Bp = nextpb()
                    for kc in range(8):
                        S.op("pe", lambda kc=kc, tt=tt, pt_=pt_, w0=w0: nc.tensor.matmul(
                            pt_[:, :], lhsT=hT[:, kc, tt * 128:(tt + 1) * 128], rhs=w0[:, kc, :],
                            start=(kc == 0), stop=(kc == 7)), reads=[BhT, Bw0], writes=[Bp], sig=(kc == 7))
                    sv, Bsv = stv[tt % 4]
                    eng = "act" if tt % 2 == 0 else "dve"
                    if eng == "act":
                        S.op("act", lambda sv=sv, pt_=pt_: nc.scalar.copy(out=sv[:], in_=pt_[:, :]), reads=[Bp], writes=[Bsv])
                    else:
                        S.op("dve", lambda sv=sv, pt_=pt_: nc.vector.tensor_copy(out=sv[:], in_=pt_[:, :]), reads=[Bp], writes=[Bsv])
                    c0 = (b0 - 4) * 512
                    S.dma("sp", VV[tt * 128:(tt + 1) * 128, c0:c0 + 512], sv[:], reads=[Bsv], writes=[BVV])
                continue
            need_ctx = (not last) or part in (0, 1)
            for cch in range(4):
                fc = (b0 % 2) * 4 + cch
                if part in (1, 3):
                    w1_, Bw1 = wb[slot_of[jb[1]]]
                    so, Bso = st16[s16i[0] % 2]
                    s16i[0] += 1
                    for (tok0, n, isctx) in token_chunks(False):
                        if isctx and not need_ctx:
                            continue
                        pk, Bpk = nextpb()
                        for kc in range(8):
                            S.op("pe", lambda kc=kc, pk=pk, tok0=tok0, n=n, cch=cch: nc.tensor.matmul(
                                pk[:, 0:n], lhsT=w0[:, kc, cch * 128:(cch + 1) * 128], rhs=hT[:, kc, tok0:tok0 + n],
                                start=(kc == 0), stop=(kc == 7)), reads=[BhT, Bw0], writes=[Bpk], sig=(kc == 7))
                        if isctx:
                            S.op("act", lambda pk=pk, so=so, n=n: nc.scalar.copy(out=so[:, 0:n], in_=pk[:, 0:n]),
                                 reads=[Bpk], writes=[Bso])
                            continue
                        pks, Bpks = nextpb()
                        for kc in range(8):
                            S.op("pe", lambda kc=kc, pks=pks, tok0=tok0, n=n, cch=cch: nc.tensor.matmul(
                                pks[:, 0:n], lhsT=w1_[:, kc, cch * 128:(cch + 1) * 128], rhs=hT[:, kc, tok0:tok0 + n],
                                start=(kc == 0), stop=(kc == 7)), reads=[BhT, Bw1], writes=[Bpks], sig=(kc == 7))
                        t1, Bt1 = t1s[tti[0] % 2]
                        t2, Bt2 = t2s[tti[0] % 2]
                        tti[0] += 1
                        l0 = tok0 - LC
                        S.op("dve", lambda t1=t1, pk=pk, l0=l0: nc.vector.tensor_tensor(
                            out=t1[:], in0=pk[:, :], in1=ctab[:, l0:l0 + 512], op=ALU.mult), reads=[Bpk, Bctab], writes=[Bt1])
                        S.op("dve", lambda t2=t2, pks=pks, l0=l0: nc.vector.tensor_tensor(
                            out=t2[:], in0=pks[:, :], in1=stab[:, l0:l0 + 512], op=ALU.mult), reads=[Bpks, Bstab], writes=[Bt2])
                        S.op("pool", lambda t1=t1, t2=t2, so=so, tok0=tok0: nc.gpsimd.tensor_tensor(
                            out=so[:, tok0:tok0 + 512], in0=t1[:], in1=t2[:], op=ALU.add), reads=[Bt1, Bt2], writes=[Bso])
                    dst, Bdst = (KT, BKT) if part == 1 else (QT, BQT)
                    a0 = 0 if need_ctx else LC
                    S.dma("sp", dst[fc, :, a0:TOK], so[:, a0:TOK], reads=[Bso], writes=[Bdst])
                else:
                    if part == 0:
                        so, Bso = st32, Bst32
                    else:
                        so, Bso = st16[s16i[0] % 2]
                        s16i[0] += 1
                    for (tok0, n, isctx) in token_chunks(False):
                        if isctx and not need_ctx:
                            continue
                        pk, Bpk = nextpb()
                        for kc in range(8):
                            S.op("pe", lambda kc=kc, pk=pk, tok0=tok0, n=n, cch=cch: nc.tensor.matmul(
                                pk[:, 0:n], lhsT=w0[:, kc, cch * 128:(cch + 1) * 128], rhs=hT[:, kc, tok0:tok0 + n],
                                start=(kc == 0), stop=(kc == 7)), reads=[BhT, Bw0], writes=[Bpk], sig=(kc == 7))
                        if part == 0:
                            evi[0] += 1
                            if evi[0] % 2 == 0:
                                S.op("dve", lambda pk=pk, so=so, tok0=tok0, n=n: nc.vector.tensor_copy(
                                    out=so[:, tok0:tok0 + n], in_=pk[:, 0:n]), reads=[Bpk], writes=[Bso])
                            else:
                                S.op("act", lambda pk=pk, so=so, tok0=tok0, n=n: nc.scalar.copy(
                                    out=so[:, tok0:tok0 + n], in_=pk[:, 0:n]), reads=[Bpk], writes=[Bso])
                        else:
                            fn = AF.Gelu_apprx_tanh if part == 4 else AF.Sigmoid
                            S.op("act", lambda pk=pk, so=so, tok0=tok0, n=n, fn=fn: nc.scalar.activation(
                                out=so[:, tok0:tok0 + n], in_=pk[:, 0:n], func=fn), reads=[Bpk], writes=[Bso])
                    dst, Bdst = {0: (RX, BRX), 4: (GRG, BGRG), 5: (SGR, BSGR), 6: (SGA, BSGA)}[part]
                    a0 = 0 if need_ctx else LC
                    S.dma("sp", dst[fc, :, a0:TOK], so[:, a0:TOK], reads=[Bso], writes=[Bdst])
        S.barrier()
        sc.close()
        scB.close()
        if stop_here("B%d" % li):
            done = True
            break

        sc = Scope(nc, "C%d" % li)
        ktb = [sc.sb("ktb%d" % i, [128, TOK], BF16) for i in range(2)]
        qtb = [[sc.sb("qtb%d_%d" % (i, c), [128, TOK], BF16) for c in range(2)] for i in range(2)]
        vb = [sc.sb("vb%d" % i, [128, NT, 132], BF16) for i in range(2)]
        NSP = 5
        pT = [sc.sb("pT%d" % i, [128, 512], BF16) for i in range(NSP)]
        osb = [[sc.sb("osb%d_%d" % (c, j), [128, 132], F32) for j in range(4)] for c in range(2)]
        oat = [sc.sb("oat%d" % i, [128, TOK], BF16) for i in range(2)]
        dl, Bdl = sc.sb("dl", [128, 256], F32)
        lamt, Blamt = sc.sb("lamt", [128, 8], F32)
        prod, Bprod = sc.sb("prod", [128, 128], F32)
        gsbc, Bgsbc = sc.sb("gsbc", [128, 128], F32)
        smalls = [sc.sb("sm%d" % i, [128, 8], F32) for i in range(4)]
        obuf = [sc.sb("ob%d" % i, [128, 128], F32) for i in range(4)]
        tbuf = [sc.sb("tb%d" % i, [128, 128], F32) for i in range(4)]
        onb = [sc.sb("onb%d" % i, [128, 128], BF16) for i in range(4)]
        junkc, Bjunkc = sc.sb("junkc", [128, 128], F32)
        ssq, Bssq = sc.sb("ssq", [128, 12], F32)
        SP = [sc.ps("SP%d" % i, [128, 512], F32) for i in range(NSP)]
        ACCB = [sc.ps("ACC%d" % i, [128, 512], F32) for i in range(2)]
        ACC = [(ACCB[j // 2][0], ACCB[j // 2][1], (j % 2) * 256) for j in range(4)]
        PTc, BPTc = sc.ps("PTc", [128, D], BF16)
        BPTcs = [Buf("PTc%d" % i) for i in range(8)]
        if MOE_SPARSE:
            NBz = ((NT - (2 if last else 0)) + 3) // 4 + 16
            z16, Bz16 = sc.sb("z16", [128, 4096], BF16)
            S.op("pool", lambda: nc.gpsimd.memset(z16[:], 0.0), writes=[Bz16])
            XSv = XS[0:NBz * BSL, :].rearrange("(p a) d -> p (a d)", p=128)
            for a_ in range(2 * NBz):
                S.dma("act", XSv[:, a_ * 4096:(a_ + 1) * 4096], z16[:], reads=[Bz16], writes=[BXS])
        S.dma("sp", dl[:], dlam_d[li].partition_broadcast(128), writes=[Bdl])
        S.dma("sp", gsbc[:], gsub_d[li].partition_broadcast(128), writes=[Bgsbc])
        S.op("act", lambda: nc.scalar.mul(out=gsbc[:], in_=gsbc[:], mul=float(1.0 - lam_init)), reads=[Bgsbc], writes=[Bgsbc])
        S.op("dve", lambda: nc.vector.tensor_tensor(out=prod[:, 0:64], in0=dl[:, 0:64], in1=dl[:, 64:128], op=ALU.mult),
             reads=[Bdl], writes=[Bprod])
        S.op("dve", lambda: nc.vector.tensor_tensor(out=prod[:, 64:128], in0=dl[:, 128:192], in1=dl[:, 192:256], op=ALU.mult),
             reads=[Bdl], writes=[Bprod])
        S.op("dve", lambda: nc.vector.tensor_reduce(out=lamt[:, 0:1], in_=prod[:, 0:64], axis=AX.X, op=ALU.add),
             reads=[Bprod], writes=[Blamt])
        S.op("dve", lambda: nc.vector.tensor_reduce(out=lamt[:, 1:2], in_=prod[:, 64:128], axis=AX.X, op=ALU.add),
             reads=[Bprod], writes=[Blamt])
        S.op("act", lambda: nc.scalar.activation(out=lamt[:, 2:4], in_=lamt[:, 0:2], func=AF.Exp), reads=[Blamt], writes=[Blamt])
        S.op("dve", lambda: nc.vector.tensor_tensor(out=lamt[:, 4:5], in0=lamt[:, 3:4], in1=lamt[:, 2:3], op=ALU.subtract),
             reads=[Blamt], writes=[Blamt])
        S.op("dve", lambda: nc.vector.tensor_scalar(out=lamt[:, 5:6], in0=lamt[:, 4:5], scalar1=float(-lam_init), scalar2=None,
                                                     op0=ALU.add), reads=[Blamt], writes=[Blamt])
        for i in range(2):
            S.op("dve", lambda i=i: nc.vector.memset(vb[i][0][:, :, 128:132], 1.0), writes=[vb[i][1]])
            for c in range(2):
                o = 64 * (1 - c)
                S.op("pool", lambda i=i, c=c, o=o: nc.gpsimd.memset(qtb[i][c][0][o:o + 64, :], 0.0), writes=[qtb[i][c][1]])
        KTsrc = KT
        qchunks = []
        if not last:
            qchunks.append((0, LC, [0, 1]))
        for i in range(SEQ // 512):
            qchunks.append((LC + i * 512, 512, list(range(NT))))
        vsrc = VV.rearrange("(kt p) c -> p kt c", p=128)
        a0 = 0 if not last else 0
        for h in range(DBG.get("heads", 8)):
            kt_, Bkt = ktb[h % 2]
            qz = qtb[h % 2]
            v_, Bv = vb[h % 2]
            oa_, Boa = oat[h % 2]
            S.dma("sp", kt_[:], KT[h], reads=[BKT], writes=[Bkt])
            q0a = 0 if not last else LC
            for c in range(2):
                S.dma("sp", qz[c][0][c * 64:(c + 1) * 64, q0a:TOK], QT[h, c * 64:(c + 1) * 64, q0a:TOK], reads=[BQT], writes=[qz[c][1]])
            for g4 in range(0, NT, 6):
                g5 = min(NT, g4 + 6)
                S.dma("sp", v_[:, g4:g5, 0:128], vsrc[:, g4:g5, h * 128:(h + 1) * 128],
                      reads=[BVV], writes=[Bv])
            units = []
            for (q0, nq, kts) in qchunks[:DBG.get("nqch", 99)]:
                for c in range(2):
                    for ki, kt_i in enumerate(kts):
                        units.append((q0, nq, c, kt_i, ki == 0, ki == len(kts) - 1))

            def emit_qk(ui):
                q0, nq, c, kt_i, first, lastk = units[ui]
                sp_, Bsp = SP[ui % NSP]
                S.op("pe", lambda: nc.tensor.matmul(
                    sp_[:, 0:nq], lhsT=kt_[:, kt_i * 128:(kt_i + 1) * 128],
                    rhs=qz[c][0][:, q0:q0 + nq], start=True, stop=True),
                    reads=[Bkt, qz[c][1]], writes=[Bsp], sig=True)

            for u0 in range(min(NSP - 1, len(units))):
                emit_qk(u0)
            smi = 0
            deferred = []
            for ui, (q0, nq, c, kt_i, first, lastk) in enumerate(units):
                while deferred and deferred[0][0] <= ui:
                    deferred.pop(0)[1]()
                sp_, Bsp = SP[ui % NSP]
                p_, Bp = pT[ui % NSP]
                S.op("act", lambda: nc.scalar.activation(out=p_[:, 0:nq], in_=sp_[:, 0:nq], func=AF.Exp, scale=0.125),
                     reads=[Bsp], writes=[Bp])
                nj = nq // 128
                for j in range(nj):
                    ac, Bac, co = ACC[j]
                    S.op("pe", lambda j=j, ac=ac, co=co: nc.tensor.matmul(
                        ac[:, co:co + 130], lhsT=p_[:, j * 128:(j + 1) * 128], rhs=v_[:, kt_i, 0:130],
                        start=(first and j % 2 == 0), stop=lastk, skip_group_check=True),
                        reads=[Bp, Bv], writes=[Bac], sig=(j == nj - 1))
                if ui + NSP - 1 < len(units):
                    emit_qk(ui + NSP - 1)
                if lastk:
                    for j in range(nj):
                        ac, Bac, co = ACC[j]
                        ot, Bot = osb[c][j]
                        S.op("dve", lambda ac=ac, ot=ot, co=co: nc.vector.tensor_copy(out=ot[:, 0:130], in_=ac[:, co:co + 130]),
                             reads=[Bac], writes=[Bot])
                    if c == 1:
                        def stage1(nj=nj, q0=q0):
                            for j in range(nj):
                                o1, Bo1 = osb[0][j]
                                o2, Bo2 = osb[1][j]
                                sm, Bsm = smalls[j]
                                ob, Bob = obuf[j]
                                tb, Btb = tbuf[j]
                                S.op("dve", lambda: nc.vector.reciprocal(out=sm[:, 0:1], in_=o1[:, 128:129]), reads=[Bo1], writes=[Bsm])
                                S.op("dve", lambda: nc.vector.reciprocal(out=sm[:, 1:2], in_=o2[:, 128:129]), reads=[Bo2], writes=[Bsm])
                                S.op("dve", lambda: nc.vector.tensor_tensor(out=sm[:, 2:3], in0=sm[:, 1:2], in1=lamt[:, 5:6], op=ALU.mult),
                                     reads=[Bsm, Blamt], writes=[Bsm])
                                S.op("dve", lambda: nc.vector.tensor_scalar(out=tb[:], in0=o1[:, 0:128], scalar1=sm[:, 0:1], scalar2=None,
                                                                             op0=ALU.mult), reads=[Bo1, Bsm], writes=[Btb])
                                S.op("dve", lambda: nc.vector.scalar_tensor_tensor(out=ob[:], in0=o2[:, 0:128], scalar=sm[:, 2:3], in1=tb[:],
                                                                                    op0=ALU.mult, op1=ALU.add),
                                     reads=[Bo2, Bsm, Btb], writes=[Bob])
                                S.op("dve", lambda: nc.vector.tensor_tensor(out=tb[:], in0=ob[:], in1=ob[:], op=ALU.mult),
                                     reads=[Bob], writes=[Btb])
                                S.op("dve", lambda j=j: nc.vector.tensor_reduce(out=ssq[:, j:j + 1], in_=tb[:], axis=AX.X, op=ALU.add),
                                     reads=[Btb], writes=[Bssq])

                        def stage2(nj=nj):
                            S.op("act", lambda: nc.scalar.activation(out=ssq[:, 4:4 + nj], in_=ssq[:, 0:nj], func=AF.Ln, scale=1.0 / 128, bias=EPS),
                                 reads=[Bssq], writes=[Bssq])
                            S.op("act", lambda: nc.scalar.activation(out=ssq[:, 8:8 + nj], in_=ssq[:, 4:4 + nj], func=AF.Exp, scale=-0.5),
                                 reads=[Bssq], writes=[Bssq])

                        def stage3(nj=nj):
                            for j in range(nj):
                                ob, Bob = obuf[j]
                                on, Bon = onb[j]
                                S.op("dve", lambda j=j: nc.vector.scalar_tensor_tensor(out=on[:], in0=ob[:], scalar=ssq[:, 8 + j:9 + j], in1=gsbc[:],
                                                                                        op0=ALU.mult, op1=ALU.mult),
                                     reads=[Bob, Bssq, Bgsbc], writes=[Bon])

                        def stage4(nj=nj):
                            for j in range(nj):
                                on, Bon = onb[j]
                                S.op("pe", lambda j=j: nc.tensor.transpose(out=PTc[:, j * 128:(j + 1) * 128], in_=on[:], identity=identb[:]),
                                     reads=[Bon, Bident], writes=[BPTc], sig=(j == nj - 1))

                        def stage5(nj=nj, q0=q0):
                            S.op("dve", lambda: nc.vector.tensor_copy(out=oa_[:, q0:q0 + nj * 128], in_=PTc[:, 0:nj * 128]),
                                 reads=[BPTc], writes=[Boa])
                        for dly, fn_ in ((1, stage1), (18, stage2), (24, stage3), (30, stage4), (34, stage5)):
                            deferred.append((ui + dly, fn_))
                        deferred.sort(key=lambda x: x[0])
            for (_, fin) in deferred:
                fin()
            S.dma("sp", OAT[h, :, q0a:TOK], oa_[:, q0a:TOK], reads=[Boa], writes=[BOAT])
        S.barrier()
        sc.close()
        if stop_here("C%d" % li):
            done = True
            break

        sc = Scope(nc, "D%d" % li)
        rxb, Brx = sc.sb("rxb", [128, TOK], F32)
        grgb = [sc.sb("grgb%d" % i, [128, TOK], BF16) for i in range(2)]
        ub, Bu = sc.sb("ub", [128, TOK], F32)
        u16, Bu16 = sc.sb("u16", [128, TOK], BF16)
        ra, Bra = sc.sb("ra", [128, TOK], F32)
        ib, Bib = sc.sb("ib", [128, TOK], F32)
        tt_, Btt = sc.sb("ttd", [128, TOK], F32)
        hf, Bhf = sc.sb("hf", [128, TOK], F32)
        hb, Bhb = sc.sb("hb", [128, TOK], F32)
        zst = [sc.sb("zst%d" % i, [128, TOK], BF16) for i in range(2)]
        cwt, Bcw = sc.sb("cwt", [128, 8, 4], F32)
        cbt, Bcb = sc.sb("cbt", [128, 8], F32)
        lwa, Blwa = sc.sb("lwa", [128, 16 * 128], BF16)
        lwx, Blwx = sc.sb("lwx", [128, 16 * 128], BF16)
        lba, Blba = sc.sb("lba", [128, 16], F32)
        lbx, Blbx = sc.sb("lbx", [128, 16], F32)
        asc, Basc = sc.sb("asc", [128, 16], F32)
        PD = [sc.ps("PD%d" % i, [128, 512], F32) for i in range(6)]
        S.dma("sp", cwt[:], cw_d[li], writes=[Bcw])
        S.dma("sp", cbt[:], cb_d[li], writes=[Bcb])
        S.dma("pool", lwa[:], lwa_d[li], writes=[Blwa])
        S.dma("pool", lwx[:], lwx_d[li], writes=[Blwx])
        S.dma("sp", lba[:], lba_d[li], writes=[Blba])
        S.dma("sp", lbx[:], lbx_d[li], writes=[Blbx])
        S.dma("sp", asc[:], llam_d[li], writes=[Basc])
        S.op("act", lambda: nc.scalar.activation(out=asc[:], in_=asc[:], func=AF.Exp, scale=-1.0), reads=[Basc], writes=[Basc])
        S.op("act", lambda: nc.scalar.activation(out=asc[:], in_=asc[:], func=AF.Ln, bias=1.0), reads=[Basc], writes=[Basc])
        S.op("dve", lambda: nc.vector.tensor_scalar(out=asc[:], in0=asc[:], scalar1=-8.0, scalar2=None, op0=ALU.mult),
             reads=[Basc], writes=[Basc])
        SEGS = [(0, LC, 0, LC)] + [(LC + i * 1024, LC + (i + 1) * 1024, LC, TOK) for i in range(4)]
        NSG = len(SEGS)
        pdi = 0
        for n in range(8):
            gg, Bgg = grgb[n % 2]
            zz, Bzz = zst[n % 2]
            S.dma("sp", rxb[:], RX[n], reads=[BRX], writes=[Brx])
            g0 = 0 if not last else LC
            S.dma("sp", gg[:, g0:TOK], GRG[n, :, g0:TOK], reads=[BGRG], writes=[Bgg])
            Bu_s = [Buf("u%d" % i) for i in range(NSG)]
            Bu16_s = [Buf("u16_%d" % i) for i in range(NSG)]
            Bra_s = [Buf("ra%d" % i) for i in range(NSG)]
            Bib_s = [Buf("ib%d" % i) for i in range(NSG)]
            Btt_s = [Buf("tt%d" % i) for i in range(NSG)]
            Bhf_s = [Buf("hf%d" % i) for i in range(NSG)]
            Bhb_s = [Buf("hb%d" % i) for i in range(NSG)]
            for B_, Bs in ((Bu, Bu_s), (Bu16, Bu16_s), (Bra, Bra_s), (Bib, Bib_s), (Btt, Btt_s), (Bhf, Bhf_s), (Bhb, Bhb_s)):
                for b_ in Bs:
                    b_.writer = B_.writer
                    b_.readers = list(B_.readers)
            for si, (s0, s1, q0_, q1_) in enumerate(SEGS):
                S.op("dve", lambda n=n, s0=s0, s1=s1: nc.vector.tensor_scalar(
                    out=ub[:, s0:s1], in0=rxb[:, s0:s1], scalar1=cwt[:, n, 2:3], scalar2=cbt[:, n:n + 1],
                    op0=ALU.mult, op1=ALU.add), reads=[Brx, Bcw, Bcb], writes=[Bu_s[si]])
                for (j, off) in ((0, -2), (1, -1), (3, 1)):
                    o0 = max(s0, q0_ - off)
                    o1 = min(s1, q1_ - off)
                    S.op("dve", lambda n=n, j=j, o0=o0, o1=o1, off=off: nc.vector.scalar_tensor_tensor(
                        out=ub[:, o0:o1], in0=rxb[:, o0 + off:o1 + off], scalar=cwt[:, n, j:j + 1],
                        in1=ub[:, o0:o1], op0=ALU.mult, op1=ALU.add), reads=[Brx, Bcw, Bu_s[si]], writes=[Bu_s[si]])
                S.op("pool", lambda s0=s0, s1=s1: nc.gpsimd.tensor_copy(out=u16[:, s0:s1], in_=ub[:, s0:s1]),
                     reads=[Bu_s[si]], writes=[Bu16_s[si]])
            for dr in range(2):
                wi = dr * 8 + n
                order = list(range(NSG)) if dr == 0 else [0, 4, 3, 2, 1]

                def st_gate(si, which):
                    global_pdi = None
                    s0, s1 = SEGS[si][0], SEGS[si][1]
                    wt, bt, dstb, Bd, Bwt, Bbt = (lwa, lba, ra, Bra_s[si], Blwa, Blba) if which == 0 else (lwx, lbx, ib, Bib_s[si], Blwx, Blbx)
                    for c0 in range(s0, s1, 512):
                        nn = min(512, s1 - c0)
                        pd_, Bpd = PD[st_gate.pdi % 6]
                        st_gate.pdi += 1
                        S.op("pe", lambda: nc.tensor.matmul(
                            pd_[:, 0:nn], lhsT=wt[:, wi * 128:(wi + 1) * 128], rhs=u16[:, c0:c0 + nn], start=True, stop=True),
                            reads=[Bwt, Bu16_s[si]], writes=[Bpd], sig=True)
                        S.op("act", lambda: nc.scalar.activation(
                            out=dstb[:, c0:c0 + nn], in_=pd_[:, 0:nn], func=AF.Sigmoid, bias=bt[:, wi:wi + 1]),
                            reads=[Bpd, Bbt], writes=[Bd])
                st_gate.pdi = pdi

                def stage(k, oi):
                    si = order[oi]
                    s0, s1 = SEGS[si][0], SEGS[si][1]
                    if k == 0:
                        st_gate(si, 0)
                    elif k == 1:
                        st_gate(si, 1)
                    elif k == 2:
                        S.op("act", lambda: nc.scalar.activation(out=ra[:, s0:s1], in_=ra[:, s0:s1], func=AF.Exp, scale=asc[:, wi:wi + 1]),
                             reads=[Bra_s[si], Basc], writes=[Bra_s[si]])
                    elif k == 3:
                        S.op("dve", lambda: nc.vector.scalar_tensor_tensor(out=tt_[:, s0:s1], in0=ra[:, s0:s1], scalar=-1.0, in1=ra[:, s0:s1],
                                                                            op0=ALU.mult, op1=ALU.mult), reads=[Bra_s[si]], writes=[Btt_s[si]])
                    elif k == 4:
                        S.op("act", lambda: nc.scalar.activation(out=tt_[:, s0:s1], in_=tt_[:, s0:s1], func=AF.Sqrt, bias=1.0),
                             reads=[Btt_s[si]], writes=[Btt_s[si]])
                    elif k == 5:
                        S.op("pool", lambda: nc.gpsimd.tensor_tensor(out=ib[:, s0:s1], in0=ib[:, s0:s1], in1=tt_[:, s0:s1], op=ALU.mult),
                             reads=[Bib_s[si], Btt_s[si]], writes=[Bib_s[si]])
                        S.op("pool", lambda: nc.gpsimd.tensor_tensor(out=ib[:, s0:s1], in0=ib[:, s0:s1], in1=ub[:, s0:s1], op=ALU.mult),
                             reads=[Bib_s[si], Bu_s[si]], writes=[Bib_s[si]])
                    elif k == 6:
                        if dr == 0:
                            if oi == 0:
                                init, rd = 0.0, []
                            else:
                                init, rd = hf[:, s0 - 1:s0], [Bhf_s[order[oi - 1]]]
                            S.op("dve", lambda: nc.vector.tensor_tensor_scan(out=hf[:, s0:s1], data0=ra[:, s0:s1], data1=ib[:, s0:s1], initial=init,
                                                                              op0=ALU.mult, op1=ALU.add),
                                 reads=[Bra_s[si], Bib_s[si]] + rd, writes=[Bhf_s[si]])
                        else:
                            if oi == 0:
                                init, rd = 0.0, []
                            elif oi == 1:
                                init, rd = hb[:, 0:1], [Bhb_s[0]]
                            else:
                                ps0 = SEGS[order[oi - 1]][0]
                                init, rd = hb[:, ps0:ps0 + 1], [Bhb_s[order[oi - 1]]]
                            S.op("dve", lambda: nc.vector.tensor_tensor_scan(out=hb[:, s0:s1][:, ::-1], data0=ra[:, s0:s1][:, ::-1],
                                                                              data1=ib[:, s0:s1][:, ::-1], initial=init,
                                                                              op0=ALU.mult, op1=ALU.add),
                                 reads=[Bra_s[si], Bib_s[si]] + rd, writes=[Bhb_s[si]])
                NST = 7
                for w in range(NSG + NST - 1):
                    for k in range(NST):
                        oi = w - k
                        if 0 <= oi < NSG:
                            stage(k, oi)
                pdi = st_gate.pdi
            for si, (s0, s1, _, _) in enumerate(SEGS):
                if last and si == 0:
                    continue
                S.op("pool", lambda s0=s0, s1=s1: nc.gpsimd.tensor_tensor(out=hf[:, s0:s1], in0=hf[:, s0:s1], in1=hb[:, s0:s1], op=ALU.add),
                     reads=[Bhf_s[si], Bhb_s[si]], writes=[Bhf_s[si]])
                S.op("pool", lambda s0=s0, s1=s1, gg=gg, zz=zz: nc.gpsimd.tensor_tensor(out=zz[:, s0:s1], in0=hf[:, s0:s1], in1=gg[:, s0:s1], op=ALU.mult),
                     reads=[Bhf_s[si], Bgg], writes=[Bzz])
            for B_, Bs in ((Bu, Bu_s), (Bu16, Bu16_s), (Bra, Bra_s), (Bib, Bib_s), (Btt, Btt_s), (Bhf, Bhf_s), (Bhb, Bhb_s)):
                B_.writer = None
                B_.readers = []
                for b_ in Bs:
                    if b_.writer is not None:
                        B_.readers.append(b_.writer)
                    B_.readers.extend(b_.readers)
            S.dma("sp", ZT[n, :, g0:TOK], zz[:, g0:TOK], reads=[Bzz], writes=[BZT])
        S.barrier()
        sc.close()
        if stop_here("D%d" % li):
            done = True
            break

        sc = Scope(nc, "E%d" % li)
        wr, Bwr = sc.sb("wr", [128, 8, D], BF16)
        wa, Bwa = sc.sb("wa", [128, 8, D], BF16)
        wo, Bwo = sc.sb("wo", [128, 8, D], BF16)
        ztb = [sc.sb("ztb%d" % i, [128, 8, 512], BF16) for i in range(2)]
        oab = [sc.sb("oab%d" % i, [128, 8, 512], BF16) for i in range(2)]
        srb = [sc.sb("srb%d" % i, [128, 8, 512], BF16) for i in range(2)]
        sab = [sc.sb("sab%d" % i, [128, 8, 512], BF16) for i in range(2)]
        xin = [sc.sb("xin%d" % i, [128, 4, D], F32) for i in range(1)]
        mT, BmT = sc.sb("mT", [128, 8, 512], BF16)
        tA = [sc.sb("tA%d" % i, [128, 512], F32) for i in range(2)]
        tB = [sc.sb("tB%d" % i, [128, 512], F32) for i in range(2)]
        tC = [sc.sb("tC%d" % i, [128, 512], F32) for i in range(2)]
        x1 = [sc.sb("x1_%d" % i, [128, D], F32) for i in range(2)]
        tmpsE = [(sc.sb("junkE%d" % i, [128, D], BF16) + sc.sb("ssE%d" % i, [128, 4], F32) + sc.sb("xnE%d" % i, [128, D], BF16))
                 for i in range(2)]
        h2st = [sc.sb("h2st%d" % i, [128, 8, 512], BF16) for i in range(2)]
        h2tk = [sc.sb("h2tk%d" % i, [128, D], BF16) for i in range(2)]

        PE_ = [sc.ps("PE%d" % i, [128, 512], F32) for i in range(6)]
        PTe = [sc.ps("PTe%d" % i, [128, D], BF16) for i in range(2)]
        for (wt, Bw, src) in ((wr, Bwr, wrp_d), (wa, Bwa, wap_d), (wo, Bwo, wo_d)):
            for hh in range(2):
                S.dma("pool", wt[:, hh * 4:(hh + 1) * 4, :], src[li].rearrange("(kc p) n -> p kc n", p=128)[:, hh * 4:(hh + 1) * 4, :],
                      writes=[Bw])
        pei = 0
        tci = 0
        for ci, (tok0, n, isctx) in enumerate(chunks):
            jj = 1 if isctx else 0
            zt_, Bzt = ztb[ci % 2]
            oa_, Boa = oab[ci % 2]
            sr_, Bsr = srb[ci % 2]
            sa_, Bsa = sab[ci % 2]
            xi_, Bxi = xin[0]
            h2_, Bh2 = h2st[ci % 2]
            S.dma("sp", zt_[:, :, 0:n], ZT.rearrange("c p t -> p c t")[:, :, tok0:tok0 + n], reads=[BZT], writes=[Bzt])
            S.dma("sp", oa_[:, :, 0:n], OAT.rearrange("c p t -> p c t")[:, :, tok0:tok0 + n], reads=[BOAT], writes=[Boa])
            S.dma("sp", sr_[:, :, 0:n], SGR.rearrange("c p t -> p c t")[:, :, tok0:tok0 + n], reads=[BSGR], writes=[Bsr])
            S.dma("sp", sa_[:, :, 0:n], SGA.rearrange("c p t -> p c t")[:, :, tok0:tok0 + n], reads=[BSGA], writes=[Bsa])
            nt_ = n // 128
            S.dma("sp", xi_[:, 0:nt_, :], xcur[tok0:tok0 + n, :].rearrange("(j p) d -> p j d", p=128), reads=[Bxcur], writes=[Bxi])
            for dc in range(8):
                por, Bpor = PE_[pei % 6]
                pei += 1
                poa, Bpoa = PE_[pei % 6]
                pei += 1
                for kc in range(8):
                    S.op("pe", lambda kc=kc, dc=dc, por=por: nc.tensor.matmul(
                        por[:, 0:n], lhsT=wr[:, kc, dc * 128:(dc + 1) * 128], rhs=zt_[:, kc, 0:n], start=(kc == 0), stop=(kc == 7)),
                        reads=[Bwr, Bzt], writes=[Bpor], sig=(kc == 7))
                for kc in range(8):
                    S.op("pe", lambda kc=kc, dc=dc, poa=poa: nc.tensor.matmul(
                        poa[:, 0:n], lhsT=wa[:, kc, dc * 128:(dc + 1) * 128], rhs=oa_[:, kc, 0:n], start=(kc == 0), stop=(kc == 7)),
                        reads=[Bwa, Boa], writes=[Bpoa], sig=(kc == 7))
                ta, Bta = tA[dc % 2]
                tb_, Btb_ = tB[dc % 2]
                S.op("dve", lambda dc=dc, ta=ta, por=por: nc.vector.tensor_tensor(out=ta[:, 0:n], in0=por[:, 0:n], in1=sr_[:, dc, 0:n], op=ALU.mult),
                     reads=[Bpor, Bsr], writes=[Bta])
                S.op("dve", lambda dc=dc, tb_=tb_, poa=poa: nc.vector.tensor_tensor(out=tb_[:, 0:n], in0=poa[:, 0:n], in1=sa_[:, dc, 0:n], op=ALU.mult),
                     reads=[Bpoa, Bsa], writes=[Btb_])
                S.op("pool", lambda dc=dc, ta=ta, tb_=tb_: nc.gpsimd.tensor_tensor(out=mT[:, dc, 0:n], in0=ta[:, 0:n], in1=tb_[:, 0:n], op=ALU.add),
                     reads=[Bta, Btb_], writes=[BmT])
            for j in range(nt_):
                tile_i = tok0 // 128 + j
                x1_, Bx1 = x1[tile_i % 2]
                for half in range(2):
                    po, Bpo = PE_[pei % 6]
                    pei += 1
                    for kc in range(8):
                        S.op("pe", lambda kc=kc, j=j, half=half, po=po: nc.tensor.matmul(
                            po[:, :], lhsT=mT[:, kc, j * 128:(j + 1) * 128], rhs=wo[:, kc, half * 512:(half + 1) * 512],
                            start=(kc == 0), stop=(kc == 7)), reads=[BmT, Bwo], writes=[Bpo], sig=(kc == 7))
                    tc_, Btc = tC[tci % 2]
                    tci += 1
                    S.op("dve", lambda half=half, po=po, tc_=tc_: nc.vector.tensor_tensor(
                        out=tc_[:], in0=po[:, :], in1=gt1bc[:, jj, half * 512:(half + 1) * 512], op=ALU.mult),
                        reads=[Bpo, Bgt1], writes=[Btc])
                    S.op("pool", lambda half=half, j=j, tc_=tc_, x1_=x1_: nc.gpsimd.tensor_tensor(
                        out=x1_[:, half * 512:(half + 1) * 512], in0=tc_[:], in1=xi_[:, j, half * 512:(half + 1) * 512], op=ALU.add),
                        reads=[Btc, Bxi], writes=[Bx1])
                S.dma("sp", XMID[tile_i * 128:(tile_i + 1) * 128, :], x1_[:], reads=[Bx1], writes=[BXMID])
                PT, BPT = PTe[tile_i % 2]
                rmsnorm_T(sc, x1_[:], Bx1, li, 1, jj, lambda kc, j=j: h2_[:, kc, j * 128:(j + 1) * 128], Bh2, PT, BPT, tmpsE[tile_i % 2])
                pr, Bpr = PE_[pei % 6]
                pei += 1
                for kc in range(8):
                    S.op("pe", lambda kc=kc, j=j, pr=pr: nc.tensor.matmul(
                        pr[:, 0:NE], lhsT=h2_[:, kc, j * 128:(j + 1) * 128], rhs=wrt[:, kc, :], start=(kc == 0), stop=(kc == 7)),
                        reads=[Bh2, Bwrt], writes=[Bpr], sig=(kc == 7))
                S.op("dve", lambda pr=pr, tile_i=tile_i: nc.vector.tensor_tensor(out=logits[:, tile_i, :], in0=pr[:, 0:NE], in1=brtbc[:], op=ALU.add),
                     reads=[Bpr, Bbrt], writes=[Blog])
                if MOE_SPARSE:
                    PT2, BPT2 = PTe[(tile_i + 1) % 2]
                    for kc in range(8):
                        S.op("pe", lambda kc=kc, j=j, PT2=PT2: nc.tensor.transpose(
                            out=PT2[:, kc * 128:(kc + 1) * 128], in_=h2_[:, kc, j * 128:(j + 1) * 128], identity=identb[:]),
                            reads=[Bh2, Bident], writes=[BPT2], sig=(kc == 7))
                    h2k, Bh2k = h2tk[tile_i % 2]
                    S.op("dve", lambda PT2=PT2, h2k=h2k: nc.vector.tensor_copy(out=h2k[:], in_=PT2[:, :]), reads=[BPT2], writes=[Bh2k])
                    S.dma("sp", H2TOK[tile_i * 128:(tile_i + 1) * 128, :], h2k[:], reads=[Bh2k], writes=[BH2TOK])
            S.dma("sp", H2T[:, :, tok0:tok0 + n], h2_[:, :, 0:n], reads=[Bh2], writes=[BH2T])
        S.barrier()
        sc.close()
        if stop_here("E%d" % li):
            done = True
            break

        t_lo = 2 if last else 0
        ntl = NT - t_lo
        sc = Scope(nc, "F%d" % li)
        if MOE_SPARSE:
            d1i, Bd1i = sc.sb("d1i", [128, NT], I32)
            d2i, Bd2i = sc.sb("d2i", [128, NT], I32)
            g12, Bg12 = sc.sb("g12", [128, 2, NT], F32)
            idxw, Bidxw = sc.sb("idxw", [128, NBMAX, 8], I32)
        scR = Scope(nc, "FR%d" % li)
        r_gm, Bgm = scR.sb("r_gm", [128, NT, 4], F32)
        r_gx, Bgx = scR.sb("r_gx", [128, NT], F32)
        r_oh, Boh = scR.sb("r_oh", [128, NT, 4], F32)
        r_ml, Bml = scR.sb("r_ml", [128, NT, NE], F32)
        r_e1, Be1 = scR.sb("r_e1", [128, NT, NE], F32)
        r_m2, Bm2 = scR.sb("r_m2", [128, NT, NE], F32)
        r_x2, Bx2 = scR.sb("r_x2", [128, NT], F32)
        r_e2, Be2 = scR.sb("r_e2", [128, NT, NE], F32)
        r_d, Bd_ = scR.sb("r_d", [128, 4, NT], F32)
        sl = slice(t_lo, NT)
        L4 = logits[:, sl, :].rearrange("p t (g e) -> p t g e", g=4)
        S.op("dve", lambda: nc.vector.tensor_reduce(out=r_gm[:, sl, :], in_=L4, axis=AX.X, op=ALU.max), reads=[Blog], writes=[Bgm])
        S.op("dve", lambda: nc.vector.tensor_reduce(out=r_gx[:, sl], in_=r_gm[:, sl, :], axis=AX.X, op=ALU.max), reads=[Bgm], writes=[Bgx])
        S.op("dve", lambda: nc.vector.tensor_tensor(out=r_oh[:, sl, :], in0=r_gm[:, sl, :],
                                                     in1=r_gx[:, sl].unsqueeze(2).to_broadcast([128, ntl, 4]), op=ALU.is_equal),
             reads=[Bgm, Bgx], writes=[Boh])
        S.op("dve", lambda: nc.vector.tensor_scalar(out=r_oh[:, sl, :], in0=r_oh[:, sl, :], scalar1=-1.0, scalar2=1e30,
                                                     op0=ALU.add, op1=ALU.mult), reads=[Boh], writes=[Boh])
        S.op("dve", lambda: nc.vector.tensor_tensor(out=r_ml[:, sl, :].rearrange("p t (g e) -> p t g e", g=4), in0=L4,
                                                     in1=r_oh[:, sl, :].unsqueeze(3).to_broadcast([128, ntl, 4, 4]), op=ALU.add),
             reads=[Blog, Boh], writes=[Bml])
        S.op("dve", lambda: nc.vector.tensor_tensor(out=r_e1[:, sl, :], in0=r_ml[:, sl, :],
                                                     in1=r_gx[:, sl].unsqueeze(2).to_broadcast([128, ntl, NE]), op=ALU.is_equal),
             reads=[Bml, Bgx], writes=[Be1])
        S.op("dve", lambda: nc.vector.scalar_tensor_tensor(out=r_m2[:, sl, :], in0=r_e1[:, sl, :], scalar=-1e30, in1=r_ml[:, sl, :],
                                                            op0=ALU.mult, op1=ALU.add), reads=[Be1, Bml], writes=[Bm2])
        S.op("dve", lambda: nc.vector.tensor_reduce(out=r_x2[:, sl], in_=r_m2[:, sl, :], axis=AX.X, op=ALU.max), reads=[Bm2], writes=[Bx2])
        S.op("dve", lambda: nc.vector.tensor_tensor(out=r_e2[:, sl, :], in0=r_m2[:, sl, :],
                                                     in1=r_x2[:, sl].unsqueeze(2).to_broadcast([128, ntl, NE]), op=ALU.is_equal),
             reads=[Bm2, Bx2], writes=[Be2])
        S.op("dve", lambda: nc.vector.tensor_tensor(out=r_d[:, 0, sl], in0=r_x2[:, sl], in1=r_gx[:, sl], op=ALU.subtract),
             reads=[Bx2, Bgx], writes=[Bd_])
        S.op("act", lambda: nc.scalar.activation(out=r_d[:, 1, sl], in_=r_d[:, 0, sl], func=AF.Exp), reads=[Bd_], writes=[Bd_])
        S.op("dve", lambda: nc.vector.tensor_scalar(out=r_d[:, 2, sl], in0=r_d[:, 1, sl], scalar1=1.0, scalar2=None, op0=ALU.add),
             reads=[Bd_], writes=[Bd_])
        S.op("dve", lambda: nc.vector.reciprocal(out=r_d[:, 2, sl], in_=r_d[:, 2, sl]), reads=[Bd_], writes=[Bd_])
        S.op("dve", lambda: nc.vector.tensor_tensor(out=r_d[:, 3, sl], in0=r_d[:, 1, sl], in1=r_d[:, 2, sl], op=ALU.mult),
             reads=[Bd_], writes=[Bd_])
        if MOE_SPARSE:
            NB = (ntl + 3) // 4 + 16
            mk, Bmk = scR.sb("mk", [128, NT, NE], F32)
            mk16, Bmk16 = scR.sb("mk16", [128, NT, NE], BF16)
            pre, Bpre = scR.sb("pre", [128, NT, NE], F32)
            tot, Btot = scR.sb("tot", [128, NT, NE], F32)
            cum, Bcum = scR.sb("cum", [128, NT, NE], F32)
            onesn, Bonesn = scR.sb("onesn", [128, 40], F32)
            sE, BsE = scR.sb("sE", [128, 8, NE], F32)
            cmpE, BcmpE = scR.sb("cmpE", [128, NE, 9], F32)
            cmpB, BcmpB = scR.sb("cmpB", [128, NBMAX, NE], F32)
            bef, Bbef = scR.sb("bef", [128, NBMAX], F32)
            idxf, Bidxf = scR.sb("idxf", [128, NBMAX, 8], F32)
            dsum, Bdsum = scR.sb("dsum", [128, 2, NT], F32)
            PRt = [scR.ps("PRt%d" % i, [128, 512], F32) for i in range(4)]
            S.op("dve", lambda: nc.vector.tensor_tensor(out=mk[:, sl, :], in0=r_e1[:, sl, :], in1=r_e2[:, sl, :], op=ALU.add),
                 reads=[Be1, Be2], writes=[Bmk])
            S.op("dve", lambda: nc.vector.tensor_copy(out=mk16[:, sl, :], in_=mk[:, sl, :]), reads=[Bmk], writes=[Bmk16])
            ncols = ntl * NE
            hc = ncols // 2
            mk16f = mk16[:, sl, :].rearrange("p t e -> p (t e)")
            pref = pre[:, sl, :].rearrange("p t e -> p (t e)")
            totf = tot[:, sl, :].rearrange("p t e -> p (t e)")
            for hh in range(2):
                c0 = hh * hc
                S.op("pe", lambda hh=hh, c0=c0: nc.tensor.matmul(PRt[hh][0][:, 0:hc], lhsT=utri16[:], rhs=mk16f[:, c0:c0 + hc], start=True, stop=True),
                     reads=[Butri, Bmk16], writes=[PRt[hh][1]], sig=True)
                S.op("pe", lambda hh=hh, c0=c0: nc.tensor.matmul(PRt[2 + hh][0][:, 0:hc], lhsT=ones16m[:], rhs=mk16f[:, c0:c0 + hc], start=True, stop=True),
                     reads=[Bonesm, Bmk16], writes=[PRt[2 + hh][1]], sig=True)
                S.op("dve", lambda hh=hh, c0=c0: nc.vector.tensor_copy(out=pref[:, c0:c0 + hc], in_=PRt[hh][0][:, 0:hc]), reads=[PRt[hh][1]], writes=[Bpre])
                S.op("act", lambda hh=hh, c0=c0: nc.scalar.copy(out=totf[:, c0:c0 + hc], in_=PRt[2 + hh][0][:, 0:hc]), reads=[PRt[2 + hh][1]], writes=[Btot])
            S.op("dve", lambda: nc.vector.memset(onesn[:], 1.0), writes=[Bonesn])
            for e in range(NE):
                S.op("dve", lambda e=e: nc.vector.tensor_tensor_scan(out=cum[:, sl, e], data0=onesn[:, 0:ntl], data1=tot[:, sl, e], initial=0.0,
                                                                      op0=ALU.mult, op1=ALU.add), reads=[Bonesn, Btot], writes=[Bcum])
            S.op("dve", lambda: nc.vector.tensor_copy(out=sE[:, 0, :], in_=cum[:, NT - 1, :]), reads=[Bcum], writes=[BsE])
            S.op("dve", lambda: nc.vector.tensor_tensor(out=tot[:, sl, :], in0=cum[:, sl, :], in1=tot[:, sl, :], op=ALU.subtract),
                 reads=[Bcum, Btot], writes=[Btot])
            S.op("dve", lambda: nc.vector.tensor_tensor(out=cmpE[:], in0=sE[:, 0, :].unsqueeze(2).to_broadcast([128, NE, 9]),
                                                         in1=thr[:, 0:18:2].unsqueeze(1).to_broadcast([128, NE, 9]), op=ALU.is_gt),
                 reads=[BsE, Bthr], writes=[BcmpE])
            S.op("dve", lambda: nc.vector.tensor_reduce(out=sE[:, 1, :], in_=cmpE[:], axis=AX.X, op=ALU.add), reads=[BcmpE], writes=[BsE])
            S.op("dve", lambda: nc.vector.tensor_scalar(out=sE[:, 2, :], in0=sE[:, 1, :], scalar1=float(BSL), scalar2=None, op0=ALU.mult),
                 reads=[BsE], writes=[BsE])
            S.op("dve", lambda: nc.vector.tensor_tensor_scan(out=sE[:, 3, :], data0=onesn[:, 0:NE], data1=sE[:, 2, :], initial=0.0,
                                                              op0=ALU.mult, op1=ALU.add), reads=[Bonesn, BsE], writes=[BsE])
            S.op("dve", lambda: nc.vector.tensor_tensor(out=sE[:, 4, :], in0=sE[:, 3, :], in1=sE[:, 2, :], op=ALU.subtract), reads=[BsE], writes=[BsE])
            S.op("dve", lambda: nc.vector.tensor_tensor(out=pre[:, sl, :], in0=pre[:, sl, :], in1=tot[:, sl, :], op=ALU.add),
                 reads=[Bpre, Btot], writes=[Bpre])
            S.op("dve", lambda: nc.vector.tensor_tensor(out=pre[:, sl, :], in0=pre[:, sl, :],
                                                         in1=sE[:, 4, :].unsqueeze(1).to_broadcast([128, ntl, NE]), op=ALU.add),
                 reads=[Bpre, BsE], writes=[Bpre])
            for k_, (re_, Bre_, di_, Bdi_) in enumerate(((r_e1, Be1, d1i, Bd1i), (r_e2, Be2, d2i, Bd2i))):
                S.op("dve", lambda re_=re_: nc.vector.tensor_tensor(out=mk[:, sl, :], in0=re_[:, sl, :], in1=pre[:, sl, :], op=ALU.mult),
                     reads=[Bre_, Bpre], writes=[Bmk])
                S.op("dve", lambda k_=k_: nc.vector.tensor_reduce(out=dsum[:, k_, sl], in_=mk[:, sl, :], axis=AX.X, op=ALU.add),
                     reads=[Bmk], writes=[Bdsum])
                S.op("dve", lambda k_=k_, di_=di_: nc.vector.tensor_copy(out=di_[:, sl], in_=dsum[:, k_, sl]), reads=[Bdsum], writes=[Bdi_])
            S.op("dve", lambda: nc.vector.tensor_copy(out=g12[:, :, sl], in_=r_d[:, 2:4, sl]), reads=[Bd_], writes=[Bg12])
            S.op("dve", lambda: nc.vector.tensor_tensor(out=cmpB[:, 0:NB, :], in0=sE[:, 3, :].unsqueeze(1).to_broadcast([128, NB, NE]),
                                                         in1=thr[:, 0:2 * NB:2].unsqueeze(2).to_broadcast([128, NB, NE]), op=ALU.is_le),
                 reads=[BsE, Bthr], writes=[BcmpB])
            S.op("dve", lambda: nc.vector.tensor_reduce(out=bef[:, 0:NB], in_=cmpB[:, 0:NB, :], axis=AX.X, op=ALU.add), reads=[BcmpB], writes=[Bbef])
            S.op("dve", lambda: nc.vector.tensor_scalar(out=bef[:, 0:NB], in0=bef[:, 0:NB], scalar1=15.0, scalar2=None, op0=ALU.min),
                 reads=[Bbef], writes=[Bbef])
            S.op("dve", lambda: nc.vector.scalar_tensor_tensor(out=idxf[:, 0:NB, :], in0=bef[:, 0:NB].unsqueeze(2).to_broadcast([128, NB, 8]), scalar=1024.0,
                                                                in1=prow[:].unsqueeze(1).to_broadcast([128, NB, 8]), op0=ALU.mult, op1=ALU.add),
                 reads=[Bbef, Bprow], writes=[Bidxf])
            S.op("dve", lambda: nc.vector.tensor_scalar(out=idxf[:, 0:NB, :], in0=idxf[:, 0:NB, :], scalar1=float(li * NE * D), scalar2=None, op0=ALU.add),
                 reads=[Bidxf], writes=[Bidxf])
            S.op("dve", lambda: nc.vector.tensor_copy(out=idxw[:, 0:NB, :], in_=idxf[:, 0:NB, :]), reads=[Bidxf], writes=[Bidxw])
        else:
            S.op("dve", lambda: nc.vector.tensor_tensor(out=r_e1[:, sl, :], in0=r_e1[:, sl, :],
                                                         in1=r_d[:, 2, sl].unsqueeze(2).to_broadcast([128, ntl, NE]), op=ALU.mult),
                 reads=[Be1, Bd_], writes=[Be1])
            S.op("dve", lambda: nc.vector.tensor_tensor(out=r_e2[:, sl, :], in0=r_e2[:, sl, :],
                                                         in1=r_d[:, 3, sl].unsqueeze(2).to_broadcast([128, ntl, NE]), op=ALU.mult),
                 reads=[Be2, Bd_], writes=[Be2])
            S.op("dve", lambda: nc.vector.tensor_tensor(out=gates[:, sl, :], in0=r_e1[:, sl, :], in1=r_e2[:, sl, :], op=ALU.add),
                 reads=[Be1, Be2], writes=[Bgates])
        if stop_here("R%d" % li):
            S.barrier()
            done = True
            break
        S.barrier()
        scR.close()
        if MOE_SPARSE:
            scS = Scope(nc, "FS%d" % li)
            h2a, Bh2a = scS.sb("h2a", [128, ntl, D], BF16)
            hsrc = H2TOK[t_lo * 128:TOK, :].rearrange("(t p) d -> p t d", p=128)
            for a_ in range(0, ntl, 8):
                b_ = min(ntl, a_ + 8)
                S.dma("sp", h2a[:, a_:b_, :], hsrc[:, a_:b_, :], reads=[BH2TOK], writes=[Bh2a])
            for t in range(t_lo, NT):
                for (di_, Bdi_) in ((d1i, Bd1i), (d2i, Bd2i)):
                    S.idma(out=XS, out_offset=bass.IndirectOffsetOnAxis(ap=di_[:, t:t + 1], axis=0), in_=h2a[:, t - t_lo, :], in_offset=None,
                           reads=[Bh2a, Bdi_], writes=[BXS])
            S.barrier()
            scS.close()
            w1b = [sc.sb("w1b%d" % i, [128, 8, D], BF16) for i in range(2)]
            w3b = [sc.sb("w3b%d" % i, [128, 8, D], BF16) for i in range(2)]
            w2b = [sc.sb("w2b%d" % i, [128, 8, D], BF16) for i in range(1)]
            xst = [sc.sb("xst%d" % i, [128, 4, D], BF16) for i in range(2)]
            xT, BxT = sc.sb("xT", [128, 8, 512], BF16)
            aT_, BaT = sc.sb("aTs", [128, 8, 512], BF16)
            tS = [sc.sb("tS%d" % i, [128, 512], F32) for i in range(2)]
            ysb = [sc.sb("ysb%d" % i, [128, 4, D], F32) for i in range(1)]
            yg = [sc.sb("yg%d" % i, [128, D], F32) for i in range(4)]
            xms = [sc.sb("xm%d" % i, [128, D], F32) for i in range(2)]
            accF, BaccF = sc.sb("accF", [128, D], F32)
            tf_, Btf = sc.sb("tFs", [128, D], F32)
            x2_, Bx2b = sc.sb("x2s", [128, D], F32)
            ssF = [sc.sb("ssF%d" % i, [128, 4], F32) for i in range(2)]
            junkF, BjunkF = sc.sb("junkF", [128, D], BF16)
            gfbc, Bgf = sc.sb("gfbc", [128, D], F32)
            S.dma("sp", gfbc[:], gfin_d.partition_broadcast(128), writes=[Bgf])
            PF = [sc.ps("PF%d" % i, [128, 512], F32) for i in range(6)]
            PTx = [sc.ps("PTx%d" % i, [128, D], BF16) for i in range(2)]
            pfi = 0
            cpi = 0
            wsrcs = [w.rearrange("l e k f -> (l e k) f") for w in (we1_d, we3_d, we2_d)]
            for jb in range(NB):
                w1_, Bw1 = w1b[jb % 2]
                w3_, Bw3 = w3b[jb % 2]
                w2_, Bw2 = w2b[0]
                for (wt, Bw, src) in ((w1_, Bw1, wsrcs[0]), (w3_, Bw3, wsrcs[1]), (w2_, Bw2, wsrcs[2])):
                    for kc in range(8):
                        S.idma(out=wt[:, kc, :], out_offset=None, in_=src,
                               in_offset=bass.IndirectOffsetOnAxis(ap=idxw[:, jb, kc:kc + 1], axis=0), reads=[Bidxw], writes=[Bw])
                for sub in range(2):
                  r0 = jb * BSL + sub * 512
                  xs_, Bxs_ = xst[sub]
                  S.dma("sp", xs_[:], XS[r0:r0 + 512, :].rearrange("(st p) d -> p st d", p=128), reads=[BXS], writes=[Bxs_])
                  for st in range(4):
                      ptx, Bptx = PTx[st % 2]
                      for kc in range(8):
                          S.op("pe", lambda kc=kc, st=st, ptx=ptx: nc.tensor.transpose(
                              out=ptx[:, kc * 128:(kc + 1) * 128], in_=xs_[:, st, kc * 128:(kc + 1) * 128], identity=identb[:]),
                              reads=[Bxs_, Bident], writes=[Bptx], sig=(kc == 7))
                      cpi += 1
                      if cpi % 2 == 0:
                          S.op("dve", lambda st=st, ptx=ptx: nc.vector.tensor_copy(
                              out=xT[:, :, st * 128:(st + 1) * 128], in_=ptx[:, :].rearrange("p (k c) -> p k c", c=128)), reads=[Bptx], writes=[BxT])
                      else:
                          S.op("act", lambda st=st, ptx=ptx: nc.scalar.copy(
                              out=xT[:, :, st * 128:(st + 1) * 128], in_=ptx[:, :].rearrange("p (k c) -> p k c", c=128)), reads=[Bptx], writes=[BxT])
                  for fc in range(8):
                      p1, Bp1 = PF[pfi % 6]
                      pfi += 1
                      p3, Bp3 = PF[pfi % 6]
                      pfi += 1
                      for kc in range(8):
                          S.op("pe", lambda kc=kc, fc=fc, p1=p1: nc.tensor.matmul(
                              p1[:, :], lhsT=w1_[:, kc, fc * 128:(fc + 1) * 128], rhs=xT[:, kc, :],
                              start=(kc == 0), stop=(kc == 7)), reads=[Bw1, BxT], writes=[Bp1], sig=(kc == 7))
                      for kc in range(8):
                          S.op("pe", lambda kc=kc, fc=fc, p3=p3: nc.tensor.matmul(
                              p3[:, :], lhsT=w3_[:, kc, fc * 128:(fc + 1) * 128], rhs=xT[:, kc, :],
                              start=(kc == 0), stop=(kc == 7)), reads=[Bw3, BxT], writes=[Bp3], sig=(kc == 7))
                      ts_, Bts = tS[fc % 2]
                      S.op("act", lambda p1=p1, ts_=ts_: nc.scalar.activation(out=ts_[:], in_=p1[:, :], func=AF.Silu), reads=[Bp1], writes=[Bts])
                      S.op("dve", lambda fc=fc, p3=p3, ts_=ts_: nc.vector.tensor_tensor(out=aT_[:, fc, :], in0=ts_[:], in1=p3[:, :], op=ALU.mult),
                           reads=[Bts, Bp3], writes=[BaT])
                  ys_, Bys_ = ysb[0]
                  for st in range(4):
                      for half in range(2):
                          py, Bpy = PF[pfi % 6]
                          pfi += 1
                          for fc in range(8):
                              S.op("pe", lambda fc=fc, st=st, half=half, py=py: nc.tensor.matmul(
                                  py[:, :], lhsT=aT_[:, fc, st * 128:(st + 1) * 128], rhs=w2_[:, fc, half * 512:(half + 1) * 512],
                                  start=(fc == 0), stop=(fc == 7)), reads=[BaT, Bw2], writes=[Bpy], sig=(fc == 7))
                          cpi += 1
                          if cpi % 2 == 0:
                              S.op("dve", lambda st=st, half=half, py=py: nc.vector.tensor_copy(out=ys_[:, st, half * 512:(half + 1) * 512], in_=py[:, :]),
                                   reads=[Bpy], writes=[Bys_])
                          else:
                              S.op("act", lambda st=st, half=half, py=py: nc.scalar.copy(out=ys_[:, st, half * 512:(half + 1) * 512], in_=py[:, :]),
                                   reads=[Bpy], writes=[Bys_])
                  S.dma("sp", YS[r0:r0 + 512, :].rearrange("(st p) d -> p st d", p=128), ys_[:], reads=[Bys_], writes=[BYS])
            S.barrier()
            for t in range(t_lo, NT):
                jj = 1 if t < 2 else 0
                y1, By1 = yg[2 * (t % 2)]
                y2, By2 = yg[2 * (t % 2) + 1]
                xm, Bxm = xms[t % 2]
                S.idma(out=y1[:], out_offset=None, in_=YS, in_offset=bass.IndirectOffsetOnAxis(ap=d1i[:, t:t + 1], axis=0), reads=[BYS, Bd1i], writes=[By1])
                S.idma(out=y2[:], out_offset=None, in_=YS, in_offset=bass.IndirectOffsetOnAxis(ap=d2i[:, t:t + 1], axis=0), reads=[BYS, Bd2i], writes=[By2])
                S.dma("sp", xm[:], XMID[t * 128:(t + 1) * 128, :], reads=[BXMID], writes=[Bxm])
                S.op("dve", lambda t=t: nc.vector.tensor_scalar(out=accF[:], in0=y1[:], scalar1=g12[:, 0, t:t + 1], scalar2=None, op0=ALU.mult),
                     reads=[By1, Bg12], writes=[BaccF])
                S.op("dve", lambda t=t: nc.vector.scalar_tensor_tensor(out=accF[:], in0=y2[:], scalar=g12[:, 1, t:t + 1], in1=accF[:],
                                                                        op0=ALU.mult, op1=ALU.add), reads=[By2, Bg12, BaccF], writes=[BaccF])
                S.op("dve", lambda jj=jj: nc.vector.tensor_tensor(out=tf_[:], in0=accF[:], in1=gt2bc[:, jj, :], op=ALU.mult),
                     reads=[BaccF, Bgt2], writes=[Btf])
                S.op("pool", lambda: nc.gpsimd.tensor_tensor(out=x2_[:], in0=tf_[:], in1=xm[:], op=ALU.add), reads=[Btf, Bxm], writes=[Bx2b])
                if not last:
                    S.dma("sp", XRES[t * 128:(t + 1) * 128, :], x2_[:], reads=[Bx2b], writes=[BXRES])
                else:
                    ss_, Bss_ = ssF[t % 2]
                    S.op("act", lambda ss_=ss_: nc.scalar.activation(out=junkF[:], in_=x2_[:], func=AF.Square, accum_out=ss_[:, 0:1]),
                         reads=[Bx2b], writes=[BjunkF, Bss_])
                    S.op("act", lambda ss_=ss_: nc.scalar.activation(out=ss_[:, 1:2], in_=ss_[:, 0:1], func=AF.Sqrt, scale=1.0 / D, bias=EPS),
                         reads=[Bss_], writes=[Bss_])
                    S.op("dve", lambda ss_=ss_: nc.vector.reciprocal(out=ss_[:, 2:3], in_=ss_[:, 1:2]), reads=[Bss_], writes=[Bss_])
                    S.op("dve", lambda ss_=ss_: nc.vector.scalar_tensor_tensor(
                        out=tf_[:], in0=x2_[:], scalar=ss_[:, 2:3], in1=gfbc[:], op0=ALU.mult, op1=ALU.mult),
                        reads=[Bx2b, Bss_, Bgf], writes=[Btf])
                    S.dma("sp", out_d[(t - 2) * 128:(t - 1) * 128, :], tf_[:], reads=[Btf], writes=[Bout])
            S.barrier()
            sc.close()
            if stop_here("F%d" % li):
                done = True
                break
            continue
        NPASS = 3
        per = (ntl + NPASS - 1) // NPASS
        w1b = [sc.sb("w1b%d" % i, [128, 8, D], BF16) for i in range(2)]
        w3b = [sc.sb("w3b%d" % i, [128, 8, D], BF16) for i in range(2)]
        w2b = [sc.sb("w2b%d" % i, [128, 8, D], BF16) for i in range(1)]
        h2p, Bh2p = sc.sb("h2p", [128, 8, per * 128], BF16)
        yacc, Byacc = sc.sb("yacc", [128, per, D], F32)
        aT = [sc.sb("aT%d" % i, [128, 8, 512], BF16) for i in range(1)]
        tS = [sc.sb("tS%d" % i, [128, 512], F32) for i in range(2)]
        xm = [sc.sb("xm%d" % i, [128, D], F32) for i in range(1)]
        tF = [sc.sb("tF%d" % i, [128, D], F32) for i in range(1)]
        x2b = [sc.sb("x2b%d" % i, [128, D], F32) for i in range(1)]
        ssF = [sc.sb("ssF%d" % i, [128, 4], F32) for i in range(2)]
        junkF, BjunkF = sc.sb("junkF", [128, D], BF16)
        gfbc, Bgf = sc.sb("gfbc", [128, D], F32)
        S.dma("sp", gfbc[:], gfin_d.partition_broadcast(128), writes=[Bgf])
        PF = [sc.ps("PF%d" % i, [128, 512], F32) for i in range(8)]
        pfi = 0
        wli = 0
        for ps_ in range(NPASS):
            ta_ = t_lo + ps_ * per
            tb2 = min(NT, ta_ + per)
            npt = tb2 - ta_
            if npt <= 0:
                continue
            S.dma("sp", h2p[:, :, 0:npt * 128], H2T[:, :, ta_ * 128:tb2 * 128], reads=[BH2T], writes=[Bh2p])
            pchunks = []
            t = 0
            while t < npt:
                nn = min(4, npt - t)
                pchunks.append((t, nn))
                t += nn
            for e in range(NE):
                w1_, Bw1 = w1b[wli % 2]
                w3_, Bw3 = w3b[wli % 2]
                w2_, Bw2 = w2b[0]
                wli += 1
                for (wt, Bw, src) in ((w1_, Bw1, we1_d), (w3_, Bw3, we3_d), (w2_, Bw2, we2_d)):
                    for hh in range(2):
                        S.dma("pool", wt[:, hh * 4:(hh + 1) * 4, :],
                              src[li, e].rearrange("(kc p) n -> p kc n", p=128)[:, hh * 4:(hh + 1) * 4, :], writes=[Bw])
                for cidx, (t0, nn) in enumerate(pchunks):
                    n = nn * 128
                    c0 = t0 * 128
                    at_, Bat = aT[0]
                    for fc in range(8):
                        p1, Bp1 = PF[pfi % 8]
                        pfi += 1
                        p3, Bp3 = PF[pfi % 8]
                        pfi += 1
                        for kc in range(8):
                            S.op("pe", lambda kc=kc, fc=fc, p1=p1: nc.tensor.matmul(
                                p1[:, 0:n], lhsT=w1_[:, kc, fc * 128:(fc + 1) * 128], rhs=h2p[:, kc, c0:c0 + n],
                                start=(kc == 0), stop=(kc == 7)), reads=[Bw1, Bh2p], writes=[Bp1], sig=(kc == 7))
                        for kc in range(8):
                            S.op("pe", lambda kc=kc, fc=fc, p3=p3: nc.tensor.matmul(
                                p3[:, 0:n], lhsT=w3_[:, kc, fc * 128:(fc + 1) * 128], rhs=h2p[:, kc, c0:c0 + n],
                                start=(kc == 0), stop=(kc == 7)), reads=[Bw3, Bh2p], writes=[Bp3], sig=(kc == 7))
                        ts_, Bts = tS[fc % 2]
                        S.op("act", lambda p1=p1, ts_=ts_: nc.scalar.activation(out=ts_[:, 0:n], in_=p1[:, 0:n], func=AF.Silu),
                             reads=[Bp1], writes=[Bts])
                        S.op("dve", lambda fc=fc, p3=p3, ts_=ts_: nc.vector.tensor_tensor(out=at_[:, fc, 0:n], in0=ts_[:, 0:n], in1=p3[:, 0:n], op=ALU.mult),
                             reads=[Bts, Bp3], writes=[Bat])
                    for j in range(nn):
                        lt = t0 + j
                        gt_ = ta_ + lt
                        for half in range(2):
                            py, Bpy = PF[pfi % 8]
                            pfi += 1
                            for fc in range(8):
                                S.op("pe", lambda fc=fc, j=j, half=half, py=py: nc.tensor.matmul(
                                    py[:, :], lhsT=at_[:, fc, j * 128:(j + 1) * 128], rhs=w2_[:, fc, half * 512:(half + 1) * 512],
                                    start=(fc == 0), stop=(fc == 7)), reads=[Bat, Bw2], writes=[Bpy], sig=(fc == 7))
                            if e == 0:
                                S.op("dve", lambda py=py, lt=lt, gt_=gt_, half=half: nc.vector.tensor_scalar(
                                    out=yacc[:, lt, half * 512:(half + 1) * 512], in0=py[:, :], scalar1=gates[:, gt_, e:e + 1], scalar2=None,
                                    op0=ALU.mult), reads=[Bpy, Bgates], writes=[Byacc])
                            else:
                                S.op("dve", lambda py=py, lt=lt, gt_=gt_, half=half, e=e: nc.vector.scalar_tensor_tensor(
                                    out=yacc[:, lt, half * 512:(half + 1) * 512], in0=py[:, :], scalar=gates[:, gt_, e:e + 1],
                                    in1=yacc[:, lt, half * 512:(half + 1) * 512], op0=ALU.mult, op1=ALU.add),
                                    reads=[Bpy, Bgates, Byacc], writes=[Byacc])
            for lt in range(npt):
                gt_ = ta_ + lt
                jj = 1 if gt_ < 2 else 0
                xm_, Bxm = xm[0]
                tf_, Btf = tF[0]
                x2_, Bx2b = x2b[0]
                S.dma("sp", xm_[:], XMID[gt_ * 128:(gt_ + 1) * 128, :], reads=[BXMID], writes=[Bxm])
                S.op("dve", lambda lt=lt, tf_=tf_, jj=jj: nc.vector.tensor_tensor(out=tf_[:], in0=yacc[:, lt, :], in1=gt2bc[:, jj, :], op=ALU.mult),
                     reads=[Byacc, Bgt2], writes=[Btf])
                S.op("pool", lambda tf_=tf_, xm_=xm_, x2_=x2_: nc.gpsimd.tensor_tensor(out=x2_[:], in0=tf_[:], in1=xm_[:], op=ALU.add),
                     reads=[Btf, Bxm], writes=[Bx2b])
                if not last:
                    S.dma("sp", XRES[gt_ * 128:(gt_ + 1) * 128, :], x2_[:], reads=[Bx2b], writes=[BXRES])
                else:
                    ss_, Bss_ = ssF[lt % 2]
                    S.op("act", lambda x2_=x2_, ss_=ss_: nc.scalar.activation(out=junkF[:], in_=x2_[:], func=AF.Square, accum_out=ss_[:, 0:1]),
                         reads=[Bx2b], writes=[BjunkF, Bss_])
                    S.op("act", lambda ss_=ss_: nc.scalar.activation(out=ss_[:, 1:2], in_=ss_[:, 0:1], func=AF.Sqrt, scale=1.0 / D, bias=EPS),
                         reads=[Bss_], writes=[Bss_])
                    S.op("dve", lambda ss_=ss_: nc.vector.reciprocal(out=ss_[:, 2:3], in_=ss_[:, 1:2]), reads=[Bss_], writes=[Bss_])
                    S.op("dve", lambda x2_=x2_, ss_=ss_, tf_=tf_: nc.vector.scalar_tensor_tensor(
                        out=tf_[:], in0=x2_[:], scalar=ss_[:, 2:3], in1=gfbc[:], op0=ALU.mult, op1=ALU.mult),
                        reads=[Bx2b, Bss_, Bgf], writes=[Btf])
                    S.dma("sp", out_d[(gt_ - 2) * 128:(gt_ - 1) * 128, :], tf_[:], reads=[Btf], writes=[Bout])
        S.barrier()
        sc.close()
        if stop_here("F%d" % li):
            done = True
            break

    S.barrier()
    build.stats = dict(S.ninstr, sems=len(S.dall), ccnt=dict(S.ccnt))
    build.marks = S.marks
    return nc


def _rope_tables():
    n_pairs = 16
    inv = (np.float32(10000.0) ** (-np.arange(n_pairs, dtype=np.float32) / np.float32(n_pairs))).astype(np.float32)
    t = np.arange(SEQ)
    r = (t // 64).astype(np.float32)
    col = (t % 64).astype(np.float32)
    ang = np.concatenate([r[:, None] * inv[None, :], col[:, None] * inv[None, :]], axis=-1).astype(np.float32)
    cos = np.cos(ang).astype(np.float32)
    sin = np.sin(ang).astype(np.float32)
    p = np.arange(128)
    pair = (p % 64) // 2
    sign = np.where(p % 2 == 0, -1.0, 1.0).astype(np.float32)
    ctab = np.ascontiguousarray(cos[:, pair].T)
    stab = np.ascontiguousarray((sin[:, pair] * sign[None, :]).T)
    return ctab, stab


def prepare_shared(inp):
    f = lambda a: np.ascontiguousarray(np.asarray(a, dtype=np.float32))
    w_in = f(inp["w_in"])
    kcols = np.arange(1024, 2048)
    qcols = np.arange(3072, 4096)
    swap = lambda c: c ^ 1
    w_in_ext = np.concatenate([w_in, w_in[:, :, swap(kcols)], w_in[:, :, swap(qcols)]], axis=2)
    b_mod = f(inp["b_mod"])
    bm = b_mod.reshape(DEPTH, 6, 8, 128)[:, [0, 1, 3, 4]]
    bmodP = np.repeat(bm.transpose(0, 3, 1, 2)[..., None], 2, axis=-1)
    gn = np.stack([f(inp["g_norm1"]).reshape(DEPTH, 8, 128), f(inp["g_norm2"]).reshape(DEPTH, 8, 128)], axis=1)
    gn = np.repeat(gn.transpose(0, 3, 1, 2)[..., None], 2, axis=-1)
    ctab, stab = _rope_tables()
    cw = f(inp["conv_w"]).reshape(DEPTH, 4, 8, 128).transpose(0, 3, 2, 1)
    cb = f(inp["conv_b"]).reshape(DEPTH, 8, 128).transpose(0, 2, 1)
    lwa = f(inp["lru_wa"]).transpose(0, 3, 1, 2, 4).reshape(DEPTH, 128, 16 * 128)
    lwx = f(inp["lru_wx"]).transpose(0, 3, 1, 2, 4).reshape(DEPTH, 128, 16 * 128)
    v16 = lambda a: f(a).reshape(DEPTH, 2, 8, 128).transpose(0, 3, 1, 2).reshape(DEPTH, 128, 16)
    shared = {
        "w_mod": f(inp["w_mod"]), "b_mod": b_mod, "bmodP": f(bmodP), "gn": f(gn),
        "w_in_ext": f(w_in_ext), "ctab": ctab, "stab": stab, "ident": np.eye(128, dtype=np.float32),
        "cw": f(cw), "cb": f(cb), "lwa": f(lwa), "lwx": f(lwx),
        "lba": v16(inp["lru_ba"]), "lbx": v16(inp["lru_bx"]), "llam": v16(inp["lru_lambda"]),
        "dlam": f(inp["diff_lambda"]).reshape(DEPTH, 256), "gsub": f(inp["g_subln"]),
        "w_rnn_proj": f(inp["w_rnn_proj"]), "w_attn_proj": f(inp["w_attn_proj"]), "w_o": f(inp["w_o"]),
        "w_router": f(inp["w_router"]), "b_router": f(inp["b_router"]),
        "w_e1": f(inp["w_e1"]), "w_e3": f(inp["w_e3"]), "w_e2": f(inp["w_e2"]),
        "g_final": f(inp["g_final"]),
        "utri": np.triu(np.ones((128, 128), np.float32), 1), "onesm": np.ones((128, 128), np.float32),
        "prow": (np.arange(8)[None, :] * 128 + np.arange(128)[:, None]).astype(np.float32),
        "thr": np.tile((512.0 * np.arange(64, dtype=np.float32))[None, :], (128, 1)),
    }
    return shared


def prepare_core(inp, b):
    x = np.asarray(inp["x"], dtype=np.float32)
    ctx = np.asarray(inp["ctx"], dtype=np.float32)
    c = np.asarray(inp["c"], dtype=np.float32)
    c_ctx = np.asarray(inp["c_ctx"], dtype=np.float32)
    x_in = np.ascontiguousarray(np.concatenate([ctx[b], x[b]], axis=0))
    cc = np.stack([c[b].reshape(8, 128).T, c_ctx.reshape(8, 128).T], axis=-1)
    return {"x_in": x_in, "cc": np.ascontiguousarray(cc.astype(np.float32))}


def kernel(**inputs):
    nb = np.asarray(inputs["x"]).shape[0]
    shared = prepare_shared(inputs)
    nc = build()
    in_maps = []
    for b in range(nb):
        m = dict(shared)
        m.update(prepare_core(inputs, b))
        in_maps.append(m)
    res = run_bass_kernel_spmd(nc, in_maps, core_ids=list(range(nb)))
    return np.stack([np.asarray(r["out"], dtype=np.float32) for r in res.results], axis=0)
```
